# Optimizing a Trainium2 kernel written in Bass

```python
import jax, jax.numpy as jnp
from jax import lax
import numpy as np

D_MODEL = 1024
BATCH = 8
SEQ = 2048
DEPTH = 2

GRID_W = 64
CTX_LEN = 256
N_MIXERS = 2
EPS = 1e-6
N_ADA = 6
POOL_WINDOWS = (2, 4, 8, 16)
N_POOL_GROUPS = 4
POOL_GROUP_DIM = D_MODEL // N_POOL_GROUPS
MLSTM_HEADS = 8
QK_DIM = D_MODEL // 2
V_DIM = D_MODEL
DK = QK_DIM // MLSTM_HEADS
DV = V_DIM // MLSTM_HEADS
CHUNK = 64
IN_PROJ_DIM = 2 * QK_DIM + 2 * V_DIM + 4 * MLSTM_HEADS
N_EXPERTS = 16
CAPACITY_FACTOR = 2
EXPERT_HIDDEN = 2 * D_MODEL
N_POOL_LAYERS = (DEPTH + 1) // 2
N_MLSTM_LAYERS = DEPTH // 2

kernel_name = "hybrid_pool_mlstm_ecmoe_dit"


def _rmsnorm(x, g):
    xf = x.astype(jnp.float32)
    y = xf * lax.rsqrt(jnp.mean(xf * xf, axis=-1, keepdims=True) + EPS)
    return (y * g.astype(jnp.float32)).astype(x.dtype)


def _modulate(h, shift, scale):
    return h * (1 + scale) + shift


def _box_mean(x, axis, w):
    n = x.shape[axis]
    s = jnp.cumsum(x.astype(jnp.float32), axis=axis)
    s = jnp.concatenate([jnp.zeros_like(lax.slice_in_dim(s, 0, 1, axis=axis)), s], axis=axis)
    t = np.arange(n)
    lo = np.clip(t - w // 2, 0, n)
    hi = np.clip(t + w - w // 2, 0, n)
    tot = jnp.take(s, hi, axis=axis) - jnp.take(s, lo, axis=axis)
    cshape = [1] * x.ndim
    cshape[axis] = n
    cnt = jnp.asarray((hi - lo).astype(np.float32)).reshape(cshape)
    return (tot / cnt).astype(x.dtype)


def _pool_mixer(hx, hc, w_pool, scale, rows):
    B, L, D = hx.shape
    g = hx.reshape(B, rows, GRID_W, N_POOL_GROUPS, POOL_GROUP_DIM)
    pooled = jnp.stack([_box_mean(_box_mean(g[..., j, :], 2, w), 1, w)
                        for j, w in enumerate(POOL_WINDOWS)], axis=3)
    yx = jnp.einsum('brwgc,gcd->brwgd', pooled - g, w_pool).reshape(B, L, D) * scale
    if hc is None:
        return yx, None
    Lc = hc.shape[1]
    gc = hc.reshape(B, Lc, N_POOL_GROUPS, POOL_GROUP_DIM)
    pooled_c = jnp.stack([_box_mean(gc[..., j, :], 1, w) for j, w in enumerate(POOL_WINDOWS)], axis=2)
    yc = jnp.einsum('blgc,gcd->blgd', pooled_c - gc, w_pool).reshape(B, Lc, D) * scale
    return yx, yc


def _mlstm_scan(q, k, v, ig, lf, state):
    B, H, T, _ = q.shape
    nc = T // CHUNK

    def to_chunks(a):
        return jnp.moveaxis(a.reshape(a.shape[:2] + (nc, CHUNK) + a.shape[3:]), 2, 0)

    tril = jnp.asarray(np.tril(np.ones((CHUNK, CHUNK), dtype=bool)))

    def step(carry, inp):
        C, n, m = carry
        qc, kc, vc, ic, fc = inp
        b = jnp.cumsum(fc, axis=-1)
        a = b + m[..., None]
        dmat = jnp.where(tril, b[..., :, None] - b[..., None, :] + ic[..., None, :], -jnp.inf)
        mj = jnp.maximum(a, jnp.max(dmat, axis=-1))
        w_inter = jnp.exp(a - mj)
        s = jnp.einsum('bhjd,bhsd->bhjs', qc, kc) * jnp.exp(dmat - mj[..., None])
        num = jnp.einsum('bhjs,bhsv->bhjv', s, vc) + w_inter[..., None] * jnp.einsum('bhvd,bhjd->bhjv', C, qc)
        den = jnp.sum(s, axis=-1) + w_inter * jnp.einsum('bhd,bhjd->bhj', n, qc)
        h = num / jnp.maximum(jnp.abs(den), jnp.exp(-mj))[..., None]
        b_last = b[..., -1]
        gl = b_last[..., None] - b + ic
        m_new = jnp.maximum(b_last + m, jnp.max(gl, axis=-1))
        ws = jnp.exp(gl - m_new[..., None])
        decay = jnp.exp(b_last + m - m_new)
        C_new = decay[..., None, None] * C + jnp.einsum('bhs,bhsv,bhsd->bhvd', ws, vc, kc)
        n_new = decay[..., None] * n + jnp.einsum('bhs,bhsd->bhd', ws, kc)
        return (C_new, n_new, m_new), h

    state, hs = lax.scan(step, state, tuple(to_chunks(a) for a in (q, k, v, ig, lf)))
    h = jnp.moveaxis(hs, 0, 2).reshape(B, H, T, -1)
    return h, state


def _mlstm_project(h, w_in, b_gates):
    B, T, _ = h.shape
    p = h @ w_in

    def heads(a, d):
        return jnp.swapaxes(a.reshape(B, T, MLSTM_HEADS, d), 1, 2).astype(jnp.float32)

    q = heads(p[..., :QK_DIM], DK)
    k = heads(p[..., QK_DIM:2 * QK_DIM], DK) * (DK ** -0.5)
    v = heads(p[..., 2 * QK_DIM:2 * QK_DIM + V_DIM], DV)
    o = jax.nn.sigmoid(p[..., 2 * QK_DIM + V_DIM:2 * QK_DIM + 2 * V_DIM])
    gates = (p[..., 2 * QK_DIM + 2 * V_DIM:] + b_gates).astype(jnp.float32)
    gates = gates.reshape(B, T, 4, MLSTM_HEADS).transpose(2, 0, 3, 1)
    return q, k, v, o, gates


def _mlstm_out(h, o, norm_g, w_out):
    B, H, T, _ = h.shape
    h = h * lax.rsqrt(jnp.mean(h * h, axis=-1, keepdims=True) + EPS)
    h = jnp.swapaxes(h, 1, 2).reshape(B, T, V_DIM) * norm_g.astype(jnp.float32)
    return (h.astype(o.dtype) * o) @ w_out


def _mlstm_mixer(hx, hc, w_in, b_gates, norm_g, w_out, ctx_out):
    B = hx.shape[0]
    qx, kx, vx, ox, gx = _mlstm_project(hx, w_in, b_gates)
    qc, kc, vc, oc, gc = _mlstm_project(hc, w_in, b_gates)
    zero = (jnp.zeros((B, MLSTM_HEADS, DV, DK), jnp.float32),
            jnp.zeros((B, MLSTM_HEADS, DK), jnp.float32),
            jnp.zeros((B, MLSTM_HEADS), jnp.float32))
    ls = jax.nn.log_sigmoid

    def flip(a):
        return jnp.flip(a, axis=2)

    hc_f, st_f = _mlstm_scan(qc, kc, vc, gc[0], ls(gc[1]), zero)
    hx_f, _ = _mlstm_scan(qx, kx, vx, gx[0], ls(gx[1]), st_f)
    hc_b, st_b = _mlstm_scan(flip(qc), flip(kc), flip(vc), flip(gc[2]), flip(ls(gc[3])), zero)
    hx_b, _ = _mlstm_scan(flip(qx), flip(kx), flip(vx), flip(gx[2]), flip(ls(gx[3])), st_b)
    yx = _mlstm_out(hx_f + flip(hx_b), ox, norm_g, w_out)
    yc = _mlstm_out(hc_f + flip(hc_b), oc, norm_g, w_out) if ctx_out else None
    return yx, yc


def _ec_moe(h, w_router, w_gate, w_up, w_down):
    B, T, D = h.shape
    cap = CAPACITY_FACTOR * T // N_EXPERTS
    aff = jax.nn.softmax(jnp.einsum('btd,de->bte', h, w_router).astype(jnp.float32), axis=-1)
    gate, idx = lax.top_k(jnp.swapaxes(aff, 1, 2), cap)
    xs = jax.vmap(lambda hb, ib: hb[ib])(h, idx)
    hid = jax.nn.silu(jnp.einsum('becd,edf->becf', xs, w_gate)) * jnp.einsum('becd,edf->becf', xs, w_up)
    ys = jnp.einsum('becf,efd->becd', hid, w_down) * gate[..., None].astype(h.dtype)
    return jax.vmap(lambda yb, ib: jnp.zeros((T, D), yb.dtype).at[ib.reshape(-1)].add(yb.reshape(-1, D)))(ys, idx)


def setup_inputs(seed: int = 0) -> dict:
    key = jax.random.key(seed)
    ks = jax.random.split(key, 19)

    def nrm(k, shape, s):
        return jax.random.normal(k, shape, jnp.float32) * s

    D = D_MODEL
    gate_offset = jnp.repeat(jnp.array([0.0, 3.0, 0.0, 3.0], jnp.float32), MLSTM_HEADS)
    return {
        "x": nrm(ks[0], (BATCH, SEQ, D), 1.0),
        "c": nrm(ks[1], (BATCH, D), 1.0),
        "ctx": nrm(ks[2], (BATCH, CTX_LEN, D), 1.0),
        "c_ctx": nrm(ks[3], (D,), 1.0),
        "ada_w": nrm(ks[4], (DEPTH, D, N_ADA * D), 0.5 * D ** -0.5),
        "ada_b": nrm(ks[5], (DEPTH, N_ADA * D), 0.02),
        "norm_mix": 1.0 + nrm(ks[6], (DEPTH, D), 0.02),
        "norm_ffn": 1.0 + nrm(ks[7], (DEPTH, D), 0.02),
        "pool_w": nrm(ks[8], (N_POOL_LAYERS, N_POOL_GROUPS, POOL_GROUP_DIM, POOL_GROUP_DIM), POOL_GROUP_DIM ** -0.5),
        "pool_scale": 1.0 + nrm(ks[9], (N_POOL_LAYERS, D), 0.1),
        "mlstm_w_in": nrm(ks[10], (N_MLSTM_LAYERS, D, IN_PROJ_DIM), D ** -0.5),
        "mlstm_b_gates": gate_offset[None] + nrm(ks[11], (N_MLSTM_LAYERS, 4 * MLSTM_HEADS), 0.1),
        "mlstm_norm": 1.0 + nrm(ks[12], (N_MLSTM_LAYERS, V_DIM), 0.02),
        "mlstm_w_out": nrm(ks[13], (N_MLSTM_LAYERS, V_DIM, D), V_DIM ** -0.5),
        "moe_router": nrm(ks[14], (DEPTH, D, N_EXPERTS), D ** -0.5),
        "moe_w_gate": nrm(ks[15], (DEPTH, N_EXPERTS, D, EXPERT_HIDDEN), D ** -0.5),
        "moe_w_up": nrm(ks[16], (DEPTH, N_EXPERTS, D, EXPERT_HIDDEN), D ** -0.5),
        "moe_w_down": nrm(ks[17], (DEPTH, N_EXPERTS, EXPERT_HIDDEN, D), EXPERT_HIDDEN ** -0.5),
        "final_norm": 1.0 + nrm(ks[18], (D,), 0.02),
    }


def reference(x, c, ctx, c_ctx, ada_w, ada_b, norm_mix, norm_ffn, pool_w, pool_scale,
              mlstm_w_in, mlstm_b_gates, mlstm_norm, mlstm_w_out,
              moe_router, moe_w_gate, moe_w_up, moe_w_down, final_norm):
    rows = x.shape[1] // GRID_W
    for i in range(DEPTH):
        last = i == DEPTH - 1
        j = i // N_MIXERS
        mx = jnp.split((jax.nn.silu(c) @ ada_w[i] + ada_b[i])[:, None, :], N_ADA, axis=-1)
        mc = jnp.split(jax.nn.silu(c_ctx) @ ada_w[i] + ada_b[i], N_ADA, axis=-1)
        hx = _modulate(_rmsnorm(x, norm_mix[i]), mx[0], mx[1])
        if i % N_MIXERS == 0:
            hc = None if last else _modulate(_rmsnorm(ctx, norm_mix[i]), mc[0], mc[1])
            yx, yc = _pool_mixer(hx, hc, pool_w[j], pool_scale[j], rows)
        else:
            hc = _modulate(_rmsnorm(ctx, norm_mix[i]), mc[0], mc[1])
            yx, yc = _mlstm_mixer(hx, hc, mlstm_w_in[j], mlstm_b_gates[j], mlstm_norm[j], mlstm_w_out[j],
                                  ctx_out=not last)
        x = x + mx[2] * yx
        hx = _modulate(_rmsnorm(x, norm_ffn[i]), mx[3], mx[4])
        x = x + mx[5] * _ec_moe(hx, moe_router[i], moe_w_gate[i], moe_w_up[i], moe_w_down[i])
        if not last:
            ctx = ctx + mc[2] * yc
            hc = _modulate(_rmsnorm(ctx, norm_ffn[i]), mc[3], mc[4])
            ctx = ctx + mc[5] * _ec_moe(hc, moe_router[i], moe_w_gate[i], moe_w_up[i], moe_w_down[i])
    return _rmsnorm(x, final_norm)
```

```python
import numpy as np
import ml_dtypes
from contextlib import ExitStack
import concourse.bass as bass
import concourse.mybir as mybir
from concourse.bass_utils import run_bass_kernel_spmd

F32 = mybir.dt.float32
BF16 = mybir.dt.bfloat16
ALU = mybir.AluOpType
AF = mybir.ActivationFunctionType
AX = mybir.AxisListType

D = 1024
SEQ = 2048
CTX = 256
NT = 18
NL = 16
E = 16
FH = 2048
POOL_WINDOWS = (2, 4, 8, 16)
EPS = 1e-6

SAME_ENGINE_SYNC = True
ENGS = ("pe", "dve", "act", "pool", "sp")


class Res:
    __slots__ = ("name", "w", "rs", "const")

    def __init__(self, name, const=False):
        self.name = name
        self.w = None
        self.rs = []
        self.const = const


class Op:
    __slots__ = ("eng", "fn", "deps", "signal", "is_dma", "lane", "cum", "sig_idx", "lane_prev")

    def __init__(self, eng, fn, is_dma=False):
        self.eng = eng
        self.fn = fn
        self.deps = set()
        self.signal = False
        self.is_dma = is_dma
        self.lane = None
        self.cum = 0
        self.sig_idx = 0
        self.lane_prev = 0


class Prog:
    def __init__(self, nc, n_lanes=8):
        self.nc = nc
        self.ops = {e: [] for e in ENGS}
        self.last = {e: None for e in ENGS}
        self.n_lanes = n_lanes
        self.lane_rr = {"sp": 0, "pool": 0, "act": 0}
        self.lane_cum = {}
        self.lane_last = {}
        self.pending = {e: set() for e in ENGS}
        self.nres = 0

    def res(self, name=None, const=False):
        self.nres += 1
        return Res(name or f"r{self.nres}", const)

    def _track(self, op, reads, writes):
        for r in reads:
            if r.w is not None:
                op.deps.add(r.w)
            if not r.const:
                r.rs.append(op)
        for r in writes:
            if r.w is not None:
                op.deps.add(r.w)
            for o in r.rs:
                op.deps.add(o)
            r.w = op
            r.rs = []
        if self.pending[op.eng]:
            op.deps |= self.pending[op.eng]
            self.pending[op.eng] = set()
        op.deps.discard(op)

    def op(self, eng, fn, reads=(), writes=()):
        o = Op(eng, fn)
        self._track(o, reads, writes)
        self.ops[eng].append(o)
        self.last[eng] = o
        return o

    def dma(self, q, out, in_, reads=(), writes=(), **kw):
        o = Op(q, None, is_dma=True)

        def fn(e, out=out, in_=in_, kw=kw):
            return e.dma_start(out=out, in_=in_, **kw)
        o.fn = fn
        lane = (q, self.lane_rr[q] % self.n_lanes)
        self.lane_rr[q] += 1
        o.lane = lane
        o.lane_prev = self.lane_cum.get(lane, 0)
        o.cum = o.lane_prev + 1
        self.lane_cum[lane] = o.cum
        self.lane_last[lane] = o
        self._track(o, reads, writes)
        self.ops[q].append(o)
        self.last[q] = o
        return o

    def barrier(self):
        deps = set(o for o in self.last.values() if o is not None)
        deps |= set(self.lane_last.values())
        for e in ENGS:
            self.pending[e] |= deps

    def emit(self, stack):
        nc = self.nc
        for e in ENGS:
            for o in self.ops[e]:
                for d in o.deps:
                    if d.is_dma:
                        continue
                    if d.eng != o.eng or (SAME_ENGINE_SYNC and o.eng != "pe"):
                        d.signal = True
        sems = {}
        for e in ENGS:
            sems[e] = stack.enter_context(nc.semaphore(f"s_{e}"))
            k = 0
            for o in self.ops[e]:
                if o.signal and not o.is_dma:
                    k += 1
                    o.sig_idx = k
        lane_sems = {}
        for lane in self.lane_cum:
            lane_sems[lane] = stack.enter_context(nc.semaphore(f"l_{lane[0]}{lane[1]}"))

        def run(ename, eng):
            waited = {}
            for o in self.ops[ename]:
                need = {}
                for d in o.deps:
                    if d.is_dma:
                        key = ("L", d.lane)
                        val = 16 * d.cum
                    else:
                        if d.eng == ename and (ename == "pe" or not SAME_ENGINE_SYNC):
                            continue
                        key = ("E", d.eng)
                        val = d.sig_idx
                    if val > need.get(key, 0):
                        need[key] = val
                if o.is_dma and o.lane_prev > 0:
                    key = ("L", o.lane)
                    val = 16 * o.lane_prev
                    if val > need.get(key, 0):
                        need[key] = val
                for key, val in need.items():
                    if waited.get(key, 0) >= val:
                        continue
                    waited[key] = val
                    s = lane_sems[key[1]] if key[0] == "L" else sems[key[1]]
                    eng.wait_ge(s, val)
                ins = o.fn(eng)
                if o.is_dma:
                    ins.then_inc(lane_sems[o.lane], 16)
                elif o.signal:
                    ins.then_inc(sems[ename], 1)
            for lane, cum in self.lane_cum.items():
                if lane[0] == ename and waited.get(("L", lane), 0) < 16 * cum:
                    eng.wait_ge(lane_sems[lane], 16 * cum)

        with nc.Block() as block:
            @block.sync
            def _(e):
                run("sp", e)

            @block.tensor
            def _(e):
                run("pe", e)

            @block.vector
            def _(e):
                run("dve", e)

            @block.scalar
            def _(e):
                run("act", e)

            @block.gpsimd
            def _(e):
                run("pool", e)


def _pool_constants():
    blocks = {}
    blk_list = []
    lat_map = {}
    ctx_map = {}
    invcnt = np.zeros((128, NT, 4), np.float32)

    def add(mat):
        key = mat.tobytes()
        if key not in blocks:
            blocks[key] = len(blk_list)
            blk_list.append(mat)
        return blocks[key]

    r = np.arange(32)
    wv = np.arange(64)
    for j, w in enumerate(POOL_WINDOWS):
        h = w // 2
        Br = ((r[None, :] >= r[:, None] - h) & (r[None, :] < r[:, None] + h)).astype(np.float32)
        Bw = ((wv[None, :] >= wv[:, None] - h) & (wv[None, :] < wv[:, None] + h)).astype(np.float32)
        M = np.kron(Br, Bw)
        cnt = M.sum(1)
        M = M - np.diag(cnt)
        invcnt[:, :NL, j] = (1.0 / cnt).reshape(NL, 128).T
        for io in range(NL):
            for ii in range(NL):
                b = M[io * 128:(io + 1) * 128, ii * 128:(ii + 1) * 128]
                if np.any(b):
                    lat_map[(j, io, ii)] = add(np.ascontiguousarray(b.T))
        t = np.arange(CTX)
        Mc = ((t[None, :] >= t[:, None] - h) & (t[None, :] < t[:, None] + h)).astype(np.float32)
        cc = Mc.sum(1)
        Mc = Mc - np.diag(cc)
        invcnt[:, NL:, j] = (1.0 / cc).reshape(2, 128).T
        for io in range(2):
            for ii in range(2):
                b = Mc[io * 128:(io + 1) * 128, ii * 128:(ii + 1) * 128]
                if np.any(b):
                    ctx_map[(j, io, ii)] = add(np.ascontiguousarray(b.T))
    blks = np.stack(blk_list).astype(ml_dtypes.bfloat16)
    assert np.all(blks.astype(np.float32) == np.stack(blk_list))
    return blks, lat_map, ctx_map, invcnt


_PC = None


def pool_constants():
    global _PC
    if _PC is None:
        _PC = _pool_constants()
    return _PC


def host_constants():
    blks, lat_map, ctx_map, invcnt = pool_constants()
    c = {}
    c["k_pblk"] = np.ascontiguousarray(blks.transpose(1, 0, 2))
    c["k_invcnt"] = invcnt
    c["k_identb"] = np.eye(128, dtype=np.float32).astype(ml_dtypes.bfloat16)
    c["k_identf"] = np.eye(128, dtype=np.float32)
    c["k_iota"] = np.tile(np.arange(256, dtype=np.float32)[None, :], (128, 1))
    ip = np.zeros((128, 4), np.float32)
    ip[:, 0] = np.arange(128)
    ip[:, 1] = np.arange(128) + 128
    c["k_iotap"] = ip
    oh = np.zeros((16, 16, 128), np.float32)
    for e in range(16):
        oh[e, e, :] = 1.0
    c["k_onehot"] = oh.astype(ml_dtypes.bfloat16)
    c["k_ones"] = np.ones((128, 2048), np.float32)
    tri = np.zeros((128, 3, 128), np.float32)
    tri[:, 0, :] = np.triu(np.ones((128, 128), np.float32))
    tri[:, 1, :] = np.tril(np.ones((128, 128), np.float32))
    tri[:, 2, :] = 1.0
    c["k_tri"] = tri
    c["k_mask2"] = np.ascontiguousarray(np.concatenate([tri[:, 0:2, :], tri[:, 0:2, :]], axis=2))
    return c


class Builder:
    def __init__(self, cfg=None):
        self.cfg = cfg or {}
        self.nc = bass.Bass("TRN2", target_bir_lowering=False)
        self.P = Prog(self.nc)
        self.taps = {}

    def sb(self, st, name, shape, dt):
        self._nsb = getattr(self, "_nsb", 0) + 1
        return st.enter_context(self.nc.sbuf_tensor(f"{name}_{self._nsb}", list(shape), dt))

    def din(self, name, shape, dt=F32):
        return self.nc.dram_tensor(name, list(shape), dt, kind="ExternalInput").ap()

    def mm(self, out, lhsT, rhs, start, stop, reads, writes):
        self.P.op("pe", lambda e: e.matmul(out, lhsT=lhsT, rhs=rhs, start=start, stop=stop), reads, writes)

    def tr(self, out, in_, ident, reads, writes):
        self.P.op("pe", lambda e: e.transpose(out, in_, ident), reads, writes)

    def act(self, out, in_, func, reads, writes, **kw):
        self.P.op("act", lambda e: e.activation(out=out, in_=in_, func=func, **kw), reads, writes)

    def tt(self, eng, out, in0, in1, op, reads, writes):
        self.P.op(eng, lambda e: e.tensor_tensor(out=out, in0=in0, in1=in1, op=op), reads, writes)

    def ts(self, eng, out, in0, s1, s2, op0, op1, reads, writes):
        if s2 is None:
            self.P.op(eng, lambda e: e.tensor_scalar(out=out, in0=in0, scalar1=s1, scalar2=None, op0=op0), reads, writes)
        else:
            self.P.op(eng, lambda e: e.tensor_scalar(out=out, in0=in0, scalar1=s1, scalar2=s2, op0=op0, op1=op1), reads, writes)

    def stt(self, eng, out, in0, scalar, in1, op0, op1, reads, writes):
        self.P.op(eng, lambda e: e.scalar_tensor_tensor(out=out, in0=in0, scalar=scalar, in1=in1, op0=op0, op1=op1), reads, writes)

    def cp(self, eng, out, in_, reads, writes):
        if eng == "act":
            self.P.op("act", lambda e: e.copy(out=out, in_=in_), reads, writes)
        else:
            self.P.op(eng, lambda e: e.tensor_copy(out=out, in_=in_), reads, writes)

    def tap(self, name, sb_ap, shape, reads, dt=F32):
        if name not in self.cfg.get("taps", ()):
            return
        d = self.nc.dram_tensor("tap_" + name, list(shape), dt, kind="ExternalOutput").ap()
        self.P.dma("sp", d, sb_ap, reads=reads)
        self.taps[name] = "tap_" + name

    def build(self):
        nc, P, cfg = self.nc, self.P, self.cfg
        blks, lat_map, ctx_map, _ = pool_constants()
        NB = blks.shape[0]
        x_d = self.din("x", [SEQ, D])
        ctx_d = self.din("ctx", [CTX, D])
        cc_d = self.din("cc", [128, 8, 2])
        ada_w = self.din("ada_w", [2, D, 6 * D])
        ada_b = self.din("ada_b", [2, 6 * D])
        norm_mix = self.din("norm_mix", [2, D])
        norm_ffn = self.din("norm_ffn", [2, D])
        pool_w = self.din("pool_w", [1, 4, 256, 256])
        pool_scale = self.din("pool_scale", [1, D])
        w_in = self.din("mlstm_w_in", [1, D, 3104])
        b_gates = self.din("mlstm_b_gates", [1, 32])
        m_norm = self.din("mlstm_norm", [1, D])
        w_out = self.din("mlstm_w_out", [1, D, D])
        router = self.din("moe_router", [2, D, E])
        EW = cfg.get("ew", E)
        w_gate = self.din("moe_w_gate", [2, EW, D, FH])
        w_up = self.din("moe_w_up", [2, EW, D, FH])
        w_down = self.din("moe_w_down", [2, EW, FH, D])
        final_norm = self.din("final_norm", [1, D])
        k_pblk = self.din("k_pblk", [128, NB, 128], BF16)
        k_invcnt = self.din("k_invcnt", [128, NT, 4])
        k_identb = self.din("k_identb", [128, 128], BF16)
        k_identf = self.din("k_identf", [128, 128])
        k_iota = self.din("k_iota", [128, 256])
        k_iotap = self.din("k_iotap", [128, 4])
        k_onehot = self.din("k_onehot", [16, 16, 128], BF16)
        k_ones = self.din("k_ones", [128, 2048])
        k_tri = self.din("k_tri", [128, 3, 128])
        k_mask2 = self.din("k_mask2", [128, 2, 256])
        out_d = nc.dram_tensor("out", [SEQ, D], F32, kind="ExternalOutput").ap()
        modv = nc.dram_tensor("modv", [2, 6, 2, D], F32).ap()

        with ExitStack() as st:
            X = self.sb(st, "X", [128, NT, D], F32)
            rX = [P.res(f"X{i}") for i in range(NT)]
            identb = self.sb(st, "identb", [128, 128], BF16)
            identf = self.sb(st, "identf", [128, 128], F32)
            iota = self.sb(st, "iota", [128, 256], F32)
            iotap = self.sb(st, "iotap", [128, 4], F32)
            epsc = self.sb(st, "epsc", [128, 1], F32)
            stat = self.sb(st, "stat", [128, 4, NT], F32)
            rK = P.res("consts", const=True)
            r_stat = P.res("stat")
            for t_, d_ in ((identb, k_identb), (identf, k_identf), (iota, k_iota), (iotap, k_iotap)):
                P.dma("sp", t_[:], d_, writes=[rK])
            P.op("dve", lambda e: e.memset(epsc[:], EPS), writes=[rK])
            PS = [st.enter_context(nc.psum_tensor(f"ps{i}", [128, 512], F32)) for i in range(8)]
            rPS = [P.res(f"ps{i}") for i in range(8)]
            NSLOT = cfg.get("nslot", 4)
            RING = [self.sb(st, f"ring{i}", [128, 4096], BF16) for i in range(NSLOT)]
            rRING = [P.res(f"ring{i}") for i in range(NSLOT)]
            self.ring_i = 0

            def ring_next():
                i = self.ring_i % NSLOT
                self.ring_i += 1
                return RING[i], rRING[i]

            xv = x_d.rearrange("(t p) d -> p t d", p=128)
            cv = ctx_d.rearrange("(t p) d -> p t d", p=128)
            for ti in range(NL):
                P.dma("sp", X[:, ti, :], xv[:, ti, :], writes=[rX[ti]])
            for ti in range(2):
                P.dma("sp", X[:, NL + ti, :], cv[:, ti, :], writes=[rX[NL + ti]])

            with ExitStack() as s0:
                cc_t = self.sb(s0, "cc_t", [128, 8, 2], F32)
                scb = self.sb(s0, "scb", [128, 8, 2], BF16)
                modrow = self.sb(s0, "modrow", [2, 6 * D], F32)
                adab = self.sb(s0, "adab", [2, 6 * D], F32)
                vec = self.sb(s0, "vec", [2, 6, D], F32)
                grow = self.sb(s0, "grow", [2, 3, D], F32)
                r_cc, r_scb, r_mod, r_adab, r_vec, r_grow = [P.res(n) for n in "cc scb mod adab vec grow".split()]
                P.dma("sp", cc_t[:], cc_d, writes=[r_cc])
                self.act(scb[:], cc_t[:], AF.Silu, [r_cc], [r_scb])
                r_modv = P.res("modv")
                self.r_modv = r_modv
                for i in range(2):
                    for s in range(2):
                        P.dma("sp", adab[s:s + 1, :], ada_b[i:i + 1, :], writes=[r_adab])
                        P.dma("sp", grow[s:s + 1, 0, :], norm_mix[i:i + 1, :], writes=[r_grow])
                        P.dma("sp", grow[s:s + 1, 1, :], norm_ffn[i:i + 1, :], writes=[r_grow])
                        if i == 0:
                            P.dma("sp", grow[s:s + 1, 2, :], pool_scale[0:1, :], writes=[r_grow])
                    awv = ada_w[i].rearrange("(k p) n -> p k n", p=128)
                    for nb in range(12):
                        slot, rs = ring_next()
                        sl3 = slot[:].rearrange("p (k n) -> p k n", k=8)
                        P.dma("pool", sl3, awv[:, :, nb * 512:(nb + 1) * 512], writes=[rs])
                        pb = nb % 2
                        for k in range(8):
                            self.mm(PS[pb][0:2, :], scb[:, k, :], sl3[:, k, :], k == 0, k == 7, [r_scb, rs], [rPS[pb]])
                        self.tt("dve", modrow[:, nb * 512:(nb + 1) * 512], PS[pb][0:2, :], adab[:, nb * 512:(nb + 1) * 512],
                                ALU.add, [rPS[pb], r_adab], [r_mod])
                    self.stt("dve", vec[:, 0, :], modrow[:, D:2 * D], 1.0, grow[:, 0, :], ALU.add, ALU.mult, [r_mod, r_grow], [r_vec])
                    self.cp("dve", vec[:, 1, :], modrow[:, 0:D], [r_mod], [r_vec])
                    if i == 0:
                        self.tt("dve", vec[:, 2, :], modrow[:, 2 * D:3 * D], grow[:, 2, :], ALU.mult, [r_mod, r_grow], [r_vec])
                    else:
                        self.cp("dve", vec[:, 2, :], modrow[:, 2 * D:3 * D], [r_mod], [r_vec])
                    self.stt("dve", vec[:, 3, :], modrow[:, 4 * D:5 * D], 1.0, grow[:, 1, :], ALU.add, ALU.mult, [r_mod, r_grow], [r_vec])
                    self.cp("dve", vec[:, 4, :], modrow[:, 3 * D:4 * D], [r_mod], [r_vec])
                    self.cp("dve", vec[:, 5, :], modrow[:, 5 * D:6 * D], [r_mod], [r_vec])
                    P.dma("sp", modv[i].rearrange("j s d -> s j d"), vec[:], reads=[r_vec], writes=[r_modv])
                P.barrier()

            def load_vec(tile, rtile, src_row):
                P.dma("sp", tile[:], src_row.partition_broadcast(128), reads=[self.r_modv], writes=[rtile])

            def rstd_for(tiles, Hjunk):
                for ti in tiles:
                    self.act(Hjunk(ti), X[:, ti, :], AF.Square, [rX[ti]], [r_stat], accum_out=stat[:, 0, ti:ti + 1])
                self.act(stat[:, 1, :], stat[:, 0, :], AF.Sqrt, [r_stat, rK], [r_stat], scale=1.0 / D, bias=epsc[:, 0:1])
                P.op("dve", lambda e: e.reciprocal(out=stat[:, 2, :], in_=stat[:, 1, :]), [r_stat], [r_stat])

            all_tiles = list(range(NT))

            with ExitStack() as s1:
                H = self.sb(s1, "H", [128, NT, D], BF16)
                rH = [P.res(f"H{i}") for i in range(NT)]
                MV = [[self.sb(s1, f"mv{s}{j}", [128, D], F32) for j in range(2)] for s in range(2)]
                rMV = [[P.res(f"mv{s}{j}") for j in range(2)] for s in range(2)]
                tmpf = self.sb(s1, "tmpf", [128, D], F32)
                r_tmpf = P.res("tmpf")
                pblk = self.sb(s1, "pblk", [128, NB, 128], BF16)
                invc = self.sb(s1, "invc", [128, NT, 4], F32)
                wp = self.sb(s1, "wp", [128, 4, 2, 256], BF16)
                ut = self.sb(s1, "ut", [128, 2, 2, 128], BF16)
                ptmp = self.sb(s1, "ptmp", [128, 2, 256], F32)
                r_pk = P.res("poolconst")
                r_ut = [P.res("ut0"), P.res("ut1")]
                r_pt = [P.res("pt0"), P.res("pt1")]
                P.dma("sp", pblk[:], k_pblk, writes=[r_pk])
                P.dma("sp", invc[:], k_invcnt, writes=[r_pk])
                P.dma("pool", wp[:], pool_w[0].rearrange("g (k p) d -> p g k d", p=128), writes=[r_pk])
                for s in range(2):
                    load_vec(MV[s][0], rMV[s][0], modv[0, 0, s:s + 1, :])
                    load_vec(MV[s][1], rMV[s][1], modv[0, 1, s:s + 1, :])
                for ti in all_tiles:
                    self.act(H[:, ti, :], X[:, ti, :], AF.Square, [rX[ti]], [r_stat, rH[ti]], accum_out=stat[:, 0, ti:ti + 1])
                self.act(stat[:, 1, :], stat[:, 0, :], AF.Sqrt, [r_stat, rK], [r_stat], scale=1.0 / D, bias=epsc[:, 0:1])
                P.op("dve", lambda e: e.reciprocal(out=stat[:, 2, :], in_=stat[:, 1, :]), [r_stat], [r_stat])
                for ti in all_tiles:
                    s = 0 if ti < NL else 1
                    self.stt("dve", tmpf[:], X[:, ti, :], stat[:, 2, ti:ti + 1], MV[s][0][:], ALU.mult, ALU.mult,
                             [rX[ti], r_stat, rMV[s][0]], [r_tmpf])
                    self.tt("dve", H[:, ti, :], tmpf[:], MV[s][1][:], ALU.add, [r_tmpf, rMV[s][1]], [rH[ti]])
                self.tap("h0", H[:], [128, NT, D], rH, BF16)
                for s in range(2):
                    load_vec(MV[s][0], rMV[s][0], modv[0, 2, s:s + 1, :])
                cnt = 0
                for j in range(4):
                    for io in range(NT):
                        s = 0 if io < NL else 1
                        if s == 0:
                            nb_ = [(ii, lat_map[(j, io, ii)]) for ii in range(NL) if (j, io, ii) in lat_map]
                        else:
                            nb_ = [(NL + ii, ctx_map[(j, io - NL, ii)]) for ii in range(2) if (j, io - NL, ii) in ctx_map]
                        b1 = cnt % 2
                        b2 = 2 + cnt % 2
                        u = cnt % 2
                        cnt += 1
                        for c in range(2):
                            ch = 2 * j + c
                            for n, (ii, bid) in enumerate(nb_):
                                self.mm(PS[b1][:, c * 128:(c + 1) * 128], H[:, ii, ch * 128:(ch + 1) * 128], pblk[:, bid, :],
                                        n == 0, n == len(nb_) - 1, [rH[ii], r_pk], [rPS[b1]])
                        self.cp("act", ut[:, u, :, :].rearrange("p c t -> p (c t)"), PS[b1][:, 0:256], [rPS[b1]], [r_ut[u]])
                        for c in range(2):
                            self.mm(PS[b2][:, 0:256], ut[:, u, c, :], wp[:, j, c, :], c == 0, c == 1, [r_ut[u], r_pk], [rPS[b2]])
                        self.stt("dve", ptmp[:, u, :], PS[b2][:, 0:256], invc[:, io, j:j + 1], MV[s][0][:, j * 256:(j + 1) * 256],
                                 ALU.mult, ALU.mult, [rPS[b2], r_pk, rMV[s][0]], [r_pt[u]])
                        self.tt("dve", X[:, io, j * 256:(j + 1) * 256], X[:, io, j * 256:(j + 1) * 256], ptmp[:, u, :], ALU.add,
                                [r_pt[u], rX[io]], [rX[io]])
                P.barrier()
            self.tap("xmix0", X[:], [128, NT, D], rX)

            def moe(layer, with_ctx):
                tiles = all_tiles if with_ctx else list(range(NL))
                nt_ = len(tiles)
                n_exp = cfg.get("n_exp", E)
                tot = SEQ + (CTX if with_ctx else 0)
                with ExitStack() as s2:
                    H = self.sb(s2, "Hm", [128, NT, D], BF16)
                    rH = [P.res(f"Hm{i}") for i in range(NT)]
                    slotb = self.sb(s2, "slotb", [16, SEQ + CTX], BF16)
                    affTb = self.sb(s2, "affTb", [16, SEQ + CTX], BF16)
                    slotT = self.sb(s2, "slotT", [128, NT, E], F32)
                    onehot = self.sb(s2, "onehot", [16, 16, 128], BF16)
                    P.dma("sp", onehot[:], k_onehot, writes=[rK])
                    r_slotb, r_affTb, r_slotT = [P.res(n) for n in "slotb affTb slotT".split()]
                    with ExitStack() as s3:
                        MV = [[self.sb(s3, f"mw{s}{j}", [128, D], F32) for j in range(2)] for s in range(2)]
                        rMV = [[P.res(f"mw{s}{j}") for j in range(2)] for s in range(2)]
                        tmpf = self.sb(s3, "tmpg", [128, D], F32)
                        r_tmpf = P.res("tmpg")
                        wr = self.sb(s3, "wr", [128, 8, E], F32)
                        hT = self.sb(s3, "hT", [128, 8, 128], F32)
                        logit = self.sb(s3, "logit", [128, NT, E], F32)
                        aff = self.sb(s3, "aff", [128, NT, E], F32)
                        sm = self.sb(s3, "sm", [128, 4, NT], F32)
                        affT = self.sb(s3, "affT", [16, SEQ + CTX], F32)
                        slotf = self.sb(s3, "slotf", [16, SEQ + CTX], F32)
                        mx8 = self.sb(s3, "mx8", [16, 8], F32)
                        r_wr, r_hT, r_logit, r_aff, r_sm, r_affT, r_mx8, r_slotf = [
                            P.res(n) for n in "wr hT logit aff sm affT mx8 slotf".split()]
                        P.dma("sp", wr[:], router[layer].rearrange("(k p) e -> p k e", p=128), writes=[r_wr])
                        for s in range(2 if with_ctx else 1):
                            load_vec(MV[s][0], rMV[s][0], modv[layer, 3, s:s + 1, :])
                            load_vec(MV[s][1], rMV[s][1], modv[layer, 4, s:s + 1, :])
                        for ti in tiles:
                            self.act(H[:, ti, :], X[:, ti, :], AF.Square, [rX[ti]], [r_stat, rH[ti]], accum_out=stat[:, 0, ti:ti + 1])
                        self.act(stat[:, 1, :], stat[:, 0, :], AF.Sqrt, [r_stat, rK], [r_stat], scale=1.0 / D, bias=epsc[:, 0:1])
                        P.op("dve", lambda e: e.reciprocal(out=stat[:, 2, :], in_=stat[:, 1, :]), [r_stat], [r_stat])
                        for ti in tiles:
                            s = 0 if ti < NL else 1
                            self.stt("dve", tmpf[:], X[:, ti, :], stat[:, 2, ti:ti + 1], MV[s][0][:], ALU.mult, ALU.mult,
                                     [rX[ti], r_stat, rMV[s][0]], [r_tmpf])
                            self.tt("dve", tmpf[:], tmpf[:], MV[s][1][:], ALU.add, [r_tmpf, rMV[s][1]], [r_tmpf])
                            self.cp("act", H[:, ti, :], tmpf[:], [r_tmpf], [rH[ti]])
                            for k in range(8):
                                b = k // 4
                                self.tr(PS[b][:, (k % 4) * 128:(k % 4 + 1) * 128], tmpf[:, k * 128:(k + 1) * 128], identf[:],
                                        [r_tmpf, rK], [rPS[b]])
                            for b in range(2):
                                self.cp("act", hT[:, b * 4:(b + 1) * 4, :].rearrange("p k t -> p (k t)"), PS[b][:, :], [rPS[b]], [r_hT])
                            for k in range(8):
                                self.mm(PS[2][:, 0:E], hT[:, k, :], wr[:, k, :], k == 0, k == 7, [r_hT, r_wr], [rPS[2]])
                            self.cp("dve", logit[:, ti, :], PS[2][:, 0:E], [rPS[2]], [r_logit])
                        P.op("dve", lambda e: e.tensor_reduce(out=sm[:, 0, 0:nt_], in_=logit[:, 0:nt_, :], axis=AX.X, op=ALU.max),
                             [r_logit], [r_sm])
                        self.tt("dve", aff[:, 0:nt_, :], logit[:, 0:nt_, :], sm[:, 0, 0:nt_].unsqueeze(2).to_broadcast([128, nt_, E]),
                                ALU.subtract, [r_logit, r_sm], [r_aff])
                        self.act(aff[:, 0:nt_, :], aff[:, 0:nt_, :], AF.Exp, [r_aff], [r_aff])
                        P.op("dve", lambda e: e.tensor_reduce(out=sm[:, 1, 0:nt_], in_=aff[:, 0:nt_, :], axis=AX.X, op=ALU.add),
                             [r_aff], [r_sm])
                        P.op("dve", lambda e: e.reciprocal(out=sm[:, 2, 0:nt_], in_=sm[:, 1, 0:nt_]), [r_sm], [r_sm])
                        self.tt("dve", aff[:, 0:nt_, :], aff[:, 0:nt_, :], sm[:, 2, 0:nt_].unsqueeze(2).to_broadcast([128, nt_, E]),
                                ALU.mult, [r_aff, r_sm], [r_aff])
                        self.tap(f"aff{layer}", aff[:], [128, NT, E], [r_aff])
                        for g in range((nt_ + 3) // 4):
                            tl = tiles[g * 4:(g + 1) * 4]
                            for n, ti in enumerate(tl):
                                self.tr(PS[3][0:16, n * 128:(n + 1) * 128], aff[:, ti, :], identf[:], [r_aff, rK], [rPS[3]])
                            w_ = len(tl) * 128
                            self.cp("act", affT[:, g * 512:g * 512 + w_], PS[3][0:16, 0:w_], [rPS[3]], [r_affT])
                        self.cp("act", affTb[:, 0:tot], affT[:, 0:tot], [r_affT], [r_affTb])
                        groups = [(0, SEQ, 256)] + ([(SEQ, CTX, 32)] if with_ctx else [])
                        for (o0, n0, k0) in groups:
                            for it in range(k0 // 8):
                                P.op("dve", lambda e, o0=o0, n0=n0: e.max(out=mx8[:], in_=affT[:, o0:o0 + n0]), [r_affT], [r_mx8])
                                P.op("dve", lambda e, o0=o0, n0=n0: e.match_replace(out=affT[:, o0:o0 + n0], in_to_replace=mx8[:],
                                                                                   in_values=affT[:, o0:o0 + n0], imm_value=0.0),
                                     [r_affT, r_mx8], [r_affT])
                        self.ts("dve", affT[:, 0:tot], affT[:, 0:tot], 0.0, None, ALU.is_equal, None, [r_affT], [r_affT])
                        for (o0, n0, k0) in groups:
                            P.op("dve", lambda e, o0=o0, n0=n0: e.tensor_tensor_scan(out=slotf[:, o0:o0 + n0], data0=affT[:, o0:o0 + n0],
                                                                                     data1=affT[:, o0:o0 + n0], initial=0.0,
                                                                                     op0=ALU.add, op1=ALU.max),
                                 [r_affT], [r_slotf])
                        self.tt("dve", slotf[:, 0:tot], slotf[:, 0:tot], affT[:, 0:tot], ALU.mult, [r_slotf, r_affT], [r_slotf])
                        self.ts("dve", slotf[:, 0:tot], slotf[:, 0:tot], -1.0, None, ALU.add, None, [r_slotf], [r_slotf])
                        self.cp("dve", slotb[:, 0:tot], slotf[:, 0:tot], [r_slotf], [r_slotb])
                        for g in range((nt_ + 3) // 4):
                            tl = tiles[g * 4:(g + 1) * 4]
                            for n, ti in enumerate(tl):
                                self.tr(PS[3][:, n * 16:(n + 1) * 16], slotf[:, ti * 128:(ti + 1) * 128], identf[0:16, 0:16],
                                        [r_slotf, rK], [rPS[3]])
                            self.cp("act", slotT[:, g * 4:g * 4 + len(tl), :].rearrange("p t e -> p (t e)"), PS[3][:, 0:len(tl) * 16],
                                    [rPS[3]], [r_slotT])
                        self.tap(f"slotT{layer}", slotT[:], [128, NT, E], [r_slotT])
                        P.barrier()

                    with ExitStack() as s4:
                        GV = [self.sb(s4, f"gv{s}", [128, D], F32) for s in range(2)]
                        rGV = [P.res(f"gv{s}") for s in range(2)]
                        selL = self.sb(s4, "selL", [128, NL, 256], BF16)
                        selC = self.sb(s4, "selC", [128, 2, 32], BF16)
                        sgt = self.sb(s4, "sgt", [128, 2, SEQ], BF16)
                        sgtC = self.sb(s4, "sgtC", [128, CTX], BF16)
                        abro = [self.sb(s4, f"abro{i}", [128, 512], F32) for i in range(2)]
                        xst = [self.sb(s4, f"xst{i}", [128, 8, 288 if with_ctx else 256], BF16) for i in range(2)]
                        hid = self.sb(s4, "hid", [128, 2, 4, 288], BF16)
                        sg = self.sb(s4, "sg", [128, 2, 288], F32)
                        yb = self.sb(s4, "yb", [128, 2, D], BF16)
                        ybC = self.sb(s4, "ybC", [128, D], BF16)
                        r_selL, r_selC, r_sgt, r_sgtC, r_yb, r_ybC = [
                            P.res(n) for n in "selL selC sgt sgtC yb ybC".split()]
                        r_abro = [P.res("abro0"), P.res("abro1")]
                        r_xst = [P.res("xst0"), P.res("xst1")]
                        r_sg = [P.res("sg0"), P.res("sg1")]
                        r_hid = [P.res("hid0"), P.res("hid1")]
                        for s in range(2 if with_ctx else 1):
                            load_vec(GV[s], rGV[s], modv[layer, 5, s:s + 1, :])
                        S = 288 if with_ctx else 256
                        wgv = w_gate[layer]
                        wuv = w_up[layer]
                        wdv = w_down[layer]
                        halves = [(0, 128), (1, 128)] + ([(2, 32)] if with_ctx else [])
                        def sel_gather(ex):
                            xs = xst[ex % len(xst)]
                            rxs = r_xst[ex % len(xst)]
                            self.tt("dve", selL[:], iota[:].unsqueeze(1).to_broadcast([128, NL, 256]),
                                    slotT[:, 0:NL, ex:ex + 1].to_broadcast([128, NL, 256]), ALU.is_equal, [r_slotT, rK], [r_selL])
                            if with_ctx:
                                self.tt("dve", selC[:], iota[:, 0:32].unsqueeze(1).to_broadcast([128, 2, 32]),
                                        slotT[:, NL:NT, ex:ex + 1].to_broadcast([128, 2, 32]), ALU.is_equal, [r_slotT, rK], [r_selC])
                            for k in range(8):
                                b = k % 2
                                for ti in range(NL):
                                    self.mm(PS[b][:, 0:256], H[:, ti, k * 128:(k + 1) * 128], selL[:, ti, :], ti == 0, ti == NL - 1,
                                            [rH[ti], r_selL], [rPS[b]])
                                if with_ctx:
                                    for tc in range(2):
                                        self.mm(PS[b][:, 256:288], H[:, NL + tc, k * 128:(k + 1) * 128], selC[:, tc, :], tc == 0, tc == 1,
                                                [rH[NL + tc], r_selC], [rPS[b]])
                                self.cp("act", xs[:, k, 0:S], PS[b][:, 0:S], [rPS[b]], [rxs])

                        def build_sgt(ex):
                            for tb in range(4):
                                b0 = 2 * (tb % 2)
                                ab = abro[tb % 2]
                                rab = r_abro[tb % 2]
                                self.mm(PS[b0][:, :], onehot[:, ex, :], slotb[:, tb * 512:(tb + 1) * 512], True, True, [r_slotb, rK], [rPS[b0]])
                                self.mm(PS[b0 + 1][:, :], onehot[:, ex, :], affTb[:, tb * 512:(tb + 1) * 512], True, True, [r_affTb, rK], [rPS[b0 + 1]])
                                self.cp("act", ab[:], PS[b0 + 1][:, :], [rPS[b0 + 1]], [rab])
                                for half in range(2):
                                    self.stt("dve", sgt[:, half, tb * 512:(tb + 1) * 512], PS[b0][:, :], iotap[:, half:half + 1], ab[:],
                                             ALU.is_equal, ALU.mult, [rPS[b0], rab, rK], [r_sgt])
                            if with_ctx:
                                ab = abro[0]
                                rab = r_abro[0]
                                self.mm(PS[0][:, 0:256], onehot[:, ex, :], slotb[:, SEQ:SEQ + CTX], True, True, [r_slotb, rK], [rPS[0]])
                                self.mm(PS[1][:, 0:256], onehot[:, ex, :], affTb[:, SEQ:SEQ + CTX], True, True, [r_affTb, rK], [rPS[1]])
                                self.cp("act", ab[:, 0:256], PS[1][:, 0:256], [rPS[1]], [rab])
                                self.stt("dve", sgtC[:, :], PS[0][:, 0:256], iotap[:, 0:1], ab[:, 0:256],
                                         ALU.is_equal, ALU.mult, [rPS[0], rab, rK], [r_sgtC])

                        pipelined = cfg.get("pipeline", True)
                        for ex in range(n_exp):
                            if ex == 0 or not pipelined:
                                sel_gather(ex)
                            build_sgt(ex)
                            xs = xst[ex % len(xst)]
                            rxs = r_xst[ex % len(xst)]
                            for fb in range(4):
                                hb = fb % 2
                                sg_, rg_ = ring_next()
                                g3 = sg_[:].rearrange("p (k n) -> p k n", k=8)
                                P.dma("pool", g3, wgv[ex].rearrange("(k p) n -> p k n", p=128)[:, :, fb * 512:(fb + 1) * 512], writes=[rg_])
                                su_, ru_ = ring_next()
                                u3 = su_[:].rearrange("p (k n) -> p k n", k=8)
                                P.dma("pool", u3, wuv[ex].rearrange("(k p) n -> p k n", p=128)[:, :, fb * 512:(fb + 1) * 512], writes=[ru_])
                                sd_, rd_ = ring_next()
                                d3 = sd_[:].rearrange("p (k n) -> p k n", k=4)
                                P.dma("pool", d3, wdv[ex].rearrange("(k p) n -> p k n", p=128)[:, fb * 4:(fb + 1) * 4, :], writes=[rd_])
                                for fc in range(4):
                                    for k in range(8):
                                        self.mm(PS[2][:, 0:S], g3[:, k, fc * 128:(fc + 1) * 128], xs[:, k, 0:S], k == 0, k == 7,
                                                [rg_, rxs], [rPS[2]])
                                    self.act(sg[:, fc % 2, 0:S], PS[2][:, 0:S], AF.Silu, [rPS[2]], [r_sg[fc % 2]])
                                    for k in range(8):
                                        self.mm(PS[3][:, 0:S], u3[:, k, fc * 128:(fc + 1) * 128], xs[:, k, 0:S], k == 0, k == 7,
                                                [ru_, rxs], [rPS[3]])
                                    self.tt("dve", hid[:, hb, fc, 0:S], sg[:, fc % 2, 0:S], PS[3][:, 0:S], ALU.mult,
                                            [r_sg[fc % 2], rPS[3]], [r_hid[hb]])
                                if pipelined and fb == 0 and ex + 1 < n_exp:
                                    sel_gather(ex + 1)
                                for fc in range(4):
                                    f = fb * 4 + fc
                                    for (hh, hn) in halves:
                                        for dh in range(2):
                                            if hh < 2:
                                                o_ = PS[4 + hh * 2 + dh][:, :]
                                                ro_ = rPS[4 + hh * 2 + dh]
                                            else:
                                                o_ = PS[dh][0:32, :]
                                                ro_ = rPS[dh]
                                            self.mm(o_, hid[:, hb, fc, hh * 128:hh * 128 + hn], d3[:, fc, dh * 512:(dh + 1) * 512],
                                                    f == 0, f == 15, [r_hid[hb], rd_], [ro_])
                            for (hh, hn) in halves:
                                for dh in range(2):
                                    if hh < 2:
                                        self.tt("dve", yb[:, hh, dh * 512:(dh + 1) * 512], PS[4 + hh * 2 + dh][:, :], GV[0][:, dh * 512:(dh + 1) * 512],
                                                ALU.mult, [rPS[4 + hh * 2 + dh], rGV[0]], [r_yb])
                                    else:
                                        self.tt("dve", ybC[0:32, dh * 512:(dh + 1) * 512], PS[dh][0:32, :], GV[1][0:32, dh * 512:(dh + 1) * 512],
                                                ALU.mult, [rPS[dh], rGV[1]], [r_ybC])
                            n_sc = 0
                            for ti in tiles:
                                for dh in range(2):
                                    b = n_sc % 4
                                    n_sc += 1
                                    if ti < NL:
                                        for hh in range(2):
                                            self.mm(PS[b][:, :], sgt[:, hh, ti * 128:(ti + 1) * 128], yb[:, hh, dh * 512:(dh + 1) * 512],
                                                    hh == 0, hh == 1, [r_sgt, r_yb], [rPS[b]])
                                    else:
                                        tc = ti - NL
                                        self.mm(PS[b][:, :], sgtC[0:32, tc * 128:(tc + 1) * 128], ybC[0:32, dh * 512:(dh + 1) * 512],
                                                True, True, [r_sgtC, r_ybC], [rPS[b]])
                                    self.tt("dve", X[:, ti, dh * 512:(dh + 1) * 512], X[:, ti, dh * 512:(dh + 1) * 512], PS[b][:, :], ALU.add,
                                            [rPS[b], rX[ti]], [rX[ti]])
                        P.barrier()

            if cfg.get("do_moe0", True):
                moe(0, True)
            self.tap("xmoe0", X[:], [128, NT, D], rX)

            def mlstm():
                h_sc = 0.125
                with ExitStack() as m0:
                    hTm = self.sb(m0, "hTm", [128, 8, NT * 128], BF16)
                    r_hTm = [P.res(f"hTm{i}") for i in range(NT)]
                    tri = self.sb(m0, "tri", [128, 3, 128], F32)
                    mask2 = self.sb(m0, "mask2", [128, 2, 256], F32)
                    onec = self.sb(m0, "onec", [128, 1], F32)
                    EQ = self.sb(m0, "EQ", [128, NT, 2, 8], F32)
                    EK = self.sb(m0, "EK", [128, NT, 2, 8], F32)
                    EGC = self.sb(m0, "EGC", [128, NT, 2, 4], F32)
                    G1 = self.sb(m0, "G1t", [128, D], F32)
                    NG = self.sb(m0, "NGt", [128, D], F32)
                    bgt = self.sb(m0, "bgt", [128, 32], F32)
                    wgt = self.sb(m0, "wgt", [128, 8, 32], BF16)
                    rT = P.res("mconst", const=True)
                    r_gates, r_SP, r_CS, r_EQ, r_EK, r_EGt, r_EGC = [P.res(n) for n in "gates SP CS EQ EK EGt EGC".split()]
                    P.dma("sp", tri[:], k_tri, writes=[rT])
                    P.dma("sp", mask2[:], k_mask2, writes=[rT])
                    P.op("dve", lambda e: e.memset(onec[:], 1.0), writes=[rT])
                    P.dma("sp", bgt[:], b_gates[0:1, :].partition_broadcast(128), writes=[rT])
                    P.dma("pool", wgt[:], w_in[0].rearrange("(k p) n -> p k n", p=128)[:, :, 3072:3104], writes=[rT])
                    load_vec(G1, rT, modv[1, 2, 0:1, :])
                    P.dma("sp", NG[:], m_norm[0:1, :].partition_broadcast(128), writes=[rT])
                    with ExitStack() as m1:
                        MV = [[self.sb(m1, f"mz{s}{j}", [128, D], F32) for j in range(2)] for s in range(2)]
                        rMV = [[P.res(f"mz{s}{j}") for j in range(2)] for s in range(2)]
                        tmpf = self.sb(m1, "tmph", [128, D], F32)
                        r_tmpf = P.res("tmph")
                        hb = self.sb(m1, "hb", [128, 2, D], BF16)
                        r_hb = [P.res("hb0"), P.res("hb1")]
                        jb = self.sb(m1, "jb", [128, D], BF16)
                        r_jb = P.res("jb")
                        for s in range(2):
                            load_vec(MV[s][0], rMV[s][0], modv[1, 0, s:s + 1, :])
                            load_vec(MV[s][1], rMV[s][1], modv[1, 1, s:s + 1, :])
                        for ti in all_tiles:
                            self.act(jb[:], X[:, ti, :], AF.Square, [rX[ti]], [r_stat, r_jb], accum_out=stat[:, 0, ti:ti + 1])
                        self.act(stat[:, 1, :], stat[:, 0, :], AF.Sqrt, [r_stat, rK], [r_stat], scale=1.0 / D, bias=epsc[:, 0:1])
                        P.op("dve", lambda e: e.reciprocal(out=stat[:, 2, :], in_=stat[:, 1, :]), [r_stat], [r_stat])
                        for ti in all_tiles:
                            s = 0 if ti < NL else 1
                            u = ti % 2
                            self.stt("dve", tmpf[:], X[:, ti, :], stat[:, 2, ti:ti + 1], MV[s][0][:], ALU.mult, ALU.mult,
                                     [rX[ti], r_stat, rMV[s][0]], [r_tmpf])
                            self.tt("dve", hb[:, u, :], tmpf[:], MV[s][1][:], ALU.add, [r_tmpf, rMV[s][1]], [r_hb[u]])
                            psb = PS[u][:, :].bitcast(BF16)
                            for k in range(8):
                                self.tr(psb[:, k * 128:(k + 1) * 128], hb[:, u, k * 128:(k + 1) * 128], identb[:], [r_hb[u], rK], [rPS[u]])
                            self.cp("act", hTm[:, :, ti * 128:(ti + 1) * 128], psb.rearrange("p (k t) -> p k t", k=8), [rPS[u]], [r_hTm[ti]])
                        P.barrier()
                    if cfg.get("ml_stop", 9) <= 1:
                        P.barrier()
                        return
                    m2 = ExitStack()
                    gates = self.sb(m2, "gates", [128, NT, 32], F32)
                    SP = self.sb(m2, "SP", [128, NT, 2, 8], F32)
                    CS = self.sb(m2, "CS", [128, NT, 32], F32)
                    EGt = self.sb(m2, "EGt", [128, NT, 16], F32)
                    for ti in all_tiles:
                        b = 2 + ti // 9
                        off = (ti % 9) * 32
                        for k in range(8):
                            self.mm(PS[b][:, off:off + 32], hTm[:, k, ti * 128:(ti + 1) * 128], wgt[:, k, :], k == 0, k == 7,
                                    [r_hTm[ti], rT], [rPS[b]])
                    for b in range(2):
                        self.tt("dve", gates[:, 9 * b:9 * b + 9, :], PS[2 + b][:, 0:288].rearrange("p (t g) -> p t g", g=32),
                                bgt[:].unsqueeze(1).to_broadcast([128, 9, 32]), ALU.add, [rPS[2 + b], rT], [r_gates])
                    for d in range(2):
                        self.act(SP[:, :, d, :], gates[:, :, 8 + 16 * d:16 + 16 * d], AF.Exp, [r_gates], [r_SP], scale=-1.0)
                    self.act(SP[:], SP[:], AF.Ln, [r_SP, rT], [r_SP], bias=onec[:, 0:1])
                    for ti in all_tiles:
                        b = 4 + ti // 9
                        off = (ti % 9) * 32
                        self.mm(PS[b][:, off:off + 8], tri[:, 0, :], SP[:, ti, 0, :], True, True, [r_SP, rT], [rPS[b]])
                        self.mm(PS[b][:, off + 8:off + 16], tri[:, 1, :], SP[:, ti, 1, :], True, True, [r_SP, rT], [rPS[b]])
                        self.mm(PS[b][:, off + 16:off + 32], tri[:, 2, :], SP[:, ti, :, :].rearrange("p d h -> p (d h)"), True, True,
                                [r_SP, rT], [rPS[b]])
                    for b in range(2):
                        self.cp("dve", CS[:, 9 * b:9 * b + 9, :], PS[4 + b][:, 0:288].rearrange("p (t g) -> p t g", g=32), [rPS[4 + b]], [r_CS])
                    self.act(EQ[:].rearrange("p t d h -> p t (d h)"), CS[:, :, 0:16], AF.Exp, [r_CS], [r_EQ], scale=-1.0)
                    for d in range(2):
                        self.tt("dve", EK[:, :, d, :], gates[:, :, 16 * d:16 * d + 8], CS[:, :, 8 * d:8 * d + 8], ALU.add, [r_gates, r_CS], [r_EK])
                    self.act(EK[:], EK[:], AF.Exp, [r_EK], [r_EK])
                    self.ts("dve", EK[:], EK[:], h_sc, None, ALU.mult, None, [r_EK], [r_EK])
                    self.act(EGt[:], CS[:, :, 16:32], AF.Exp, [r_CS], [r_EGt], scale=-1.0)
                    eg5 = EGt[:].rearrange("p t (d g l) -> p t d g l", d=2, g=4, l=2)
                    self.cp("dve", EGC[0:64], eg5[0:64, :, :, :, 0], [r_EGt], [r_EGC])
                    self.cp("dve", EGC[64:128], eg5[64:128, :, :, :, 1], [r_EGt], [r_EGC])
                    self.tap("mgates", gates[:], [128, NT, 32], [r_gates])
                    self.tap("mEQ", EQ[:], [128, NT, 2, 8], [r_EQ])
                    self.tap("mEK", EK[:], [128, NT, 2, 8], [r_EK])
                    self.tap("mEGC", EGC[:], [128, NT, 2, 4], [r_EGC])
                    P.barrier()
                    m2.close()
                    if cfg.get("ml_stop", 9) <= 2:
                        return
                    with ExitStack() as m3:
                        Qs = self.sb(m3, "Qs", [128, NT, 128], BF16)
                        Ks = self.sb(m3, "Ks", [128, NT, 128], BF16)
                        Vh = self.sb(m3, "Vh", [128, NT, 2, 144], BF16)
                        HF = self.sb(m3, "HF", [128, NL, 256], F32)
                        r_Q = [P.res(f"Q{i}") for i in range(NT)]
                        r_V = [P.res(f"V{i}") for i in range(NT)]
                        r_HF = [P.res(f"HF{i}") for i in range(NL)]
                        qs = [self.sb(m3, f"qs{d}", [128, 128], BF16) for d in range(2)]
                        ks = [self.sb(m3, f"ks{d}", [128, 128], BF16) for d in range(2)]
                        qkT = [self.sb(m3, f"qkT{d}", [128, 3, 128], BF16) for d in range(2)]
                        ST = [self.sb(m3, f"ST{d}", [128, 2, 128], BF16) for d in range(2)]
                        Cf = [self.sb(m3, f"Cf{d}", [128, 144], F32) for d in range(2)]
                        Cb = [self.sb(m3, f"Cb{d}", [128, 144], BF16) for d in range(2)]
                        ctmp = [self.sb(m3, f"ctmp{d}", [128, 144], F32) for d in range(2)]
                        rr = [self.sb(m3, f"rr{d}", [128, 4], F32) for d in range(2)]
                        r_qs, r_ks, r_qkT, r_ST, r_Cf, r_Cb, r_ctmp, r_rr = [[P.res(f"{n}{d}") for d in range(2)]
                                                                             for n in "qs ks qkT ST Cf Cb ctmp rr".split()]
                        og = self.sb(m3, "og", [128, 2, 256], F32)
                        sq = [self.sb(m3, f"sq{i}", [128, 256], F32) for i in range(2)]
                        ss = [self.sb(m3, f"ss{i}", [128, 8], F32) for i in range(2)]
                        t1 = [self.sb(m3, "t1s", [128, 256], F32)] * 2
                        ho = [self.sb(m3, f"ho{i}", [128, 256], BF16) for i in range(2)]
                        hoT = [self.sb(m3, f"hoT{i}", [128, 2, 128], BF16) for i in range(2)]
                        y5 = [self.sb(m3, "y5s", [128, 1, 512], F32)] * 2
                        r_og = [P.res("og0"), P.res("og1")]
                        r_sq, r_ss, r_ho, r_hoT = [[P.res(f"{n}{i}") for i in range(2)] for n in "sq ss ho hoT".split()]
                        r_t1 = [P.res("t1s")] * 2
                        r_y5 = [P.res("y5s")] * 2
                        for ti in all_tiles:
                            P.op("dve", lambda e, ti=ti: e.memset(Vh[:, ti, :, :].rearrange("p h v -> p (h v)"), 0.0), writes=[r_V[ti]])
                            for hl in range(2):
                                P.op("dve", lambda e, ti=ti, hl=hl: e.memset(Vh[:, ti, hl, 128:129], 1.0), writes=[r_V[ti]])
                        w3 = w_in[0].rearrange("(k p) n -> p k n", p=128)
                        order = [[16, 17] + list(range(16)), [17, 16] + list(range(15, -1, -1))]
                        for hg in range(cfg.get("n_hg", 4)):
                            slot, rs = ring_next()
                            wqkv = slot[:].rearrange("p (k n) -> p k n", k=8)
                            P.dma("pool", wqkv[:, :, 0:128], w3[:, :, hg * 128:(hg + 1) * 128], writes=[rs])
                            P.dma("pool", wqkv[:, :, 128:256], w3[:, :, 512 + hg * 128:512 + (hg + 1) * 128], writes=[rs])
                            P.dma("pool", wqkv[:, :, 256:512], w3[:, :, 1024 + hg * 256:1024 + (hg + 1) * 256], writes=[rs])
                            pj = cfg.get("pj_upto", 9)
                            for ti in all_tiles:
                                b = 4 + ti % 2
                                if pj < 2:
                                    break
                                for k in range(8):
                                    self.mm(PS[b][:, :], hTm[:, k, ti * 128:(ti + 1) * 128], wqkv[:, k, :], k == 0, k == 7,
                                            [r_hTm[ti], rs], [rPS[b]])
                                if pj < 3:
                                    continue
                                self.cp("act", Qs[:, ti, :], PS[b][:, 0:128], [rPS[b]], [r_Q[ti]])
                                self.cp("act", Ks[:, ti, :], PS[b][:, 128:256], [rPS[b]], [r_Q[ti]])
                                if pj < 4:
                                    continue
                                for hl in range(2):
                                    self.cp("act", Vh[:, ti, hl, 0:128], PS[b][:, 256 + 128 * hl:384 + 128 * hl], [rPS[b]], [r_V[ti]])
                            for ti in range(NL):
                                P.op("dve", lambda e, ti=ti: e.memset(HF[:, ti, :], 0.0), writes=[r_HF[ti]])
                            for d in range(2):
                                if hg == 0:
                                    P.op("dve", lambda e, d=d: e.memset(qkT[d][:].rearrange("p a t -> p (a t)"), 0.0), writes=[r_qkT[d]])
                                P.op("dve", lambda e, d=d: e.memset(Cf[d][:], 0.0), writes=[r_Cf[d]])
                                P.op("dve", lambda e, d=d: e.memset(Cb[d][:], 0.0), writes=[r_Cb[d]])
                            h0 = 2 * hg
                            if cfg.get("ml_stop", 9) <= 3:
                                P.barrier()
                                return
                            for step in range(cfg.get("ml_steps", NT)):
                                for d in range(2):
                                    c = order[d][step]
                                    lat_c = c < NL
                                    self.tt("dve", ks[d][:].rearrange("p (h k) -> p h k", h=2), Ks[:, c, :].rearrange("p (h k) -> p h k", h=2),
                                            EK[:, c, d, h0:h0 + 2].unsqueeze(2).to_broadcast([128, 2, 64]), ALU.mult,
                                            [r_Q[c], r_EK], [r_ks[d]])
                                    if lat_c:
                                        self.tt("dve", qs[d][:].rearrange("p (h k) -> p h k", h=2), Qs[:, c, :].rearrange("p (h k) -> p h k", h=2),
                                                EQ[:, c, d, h0:h0 + 2].unsqueeze(2).to_broadcast([128, 2, 64]), ALU.mult,
                                                [r_Q[c], r_EQ], [r_qs[d]])
                                        psb = PS[d][:, :].bitcast(BF16)
                                        self.tr(psb[:, 0:128], qs[d][:], identb[:], [r_qs[d], rK], [rPS[d]])
                                        self.tr(psb[:, 128:256], ks[d][:], identb[:], [r_ks[d], rK], [rPS[d]])
                                        self.cp("act", qkT[d][0:64, 0, :], psb[0:64, 0:128], [rPS[d]], [r_qkT[d]])
                                        self.cp("act", qkT[d][64:128, 1, :], psb[64:128, 0:128], [rPS[d]], [r_qkT[d]])
                                        self.cp("act", qkT[d][:, 2, :], psb[:, 128:256], [rPS[d]], [r_qkT[d]])
                                        for hl in range(2):
                                            self.mm(PS[2 + d][:, hl * 128:(hl + 1) * 128], qkT[d][:, 2, :],
                                                    qkT[d][:, hl, :], True, True, [r_qkT[d]], [rPS[2 + d]])
                                        self.tt("dve", ST[d][:].rearrange("p h t -> p (h t)"), PS[2 + d][:, 0:256], mask2[:, d, :], ALU.mult,
                                                [rPS[2 + d], rT], [r_ST[d]])
                                        for hl in range(2):
                                            self.mm(PS[4 + d][:, hl * 144:hl * 144 + 129], qkT[d][:, hl, :],
                                                    Cb[d][:, 0:129], True, False, [r_qkT[d], r_Cb[d]], [rPS[4 + d]])
                                            self.mm(PS[4 + d][:, hl * 144:hl * 144 + 129], ST[d][:, hl, :], Vh[:, c, hl, 0:129], False, True,
                                                    [r_ST[d], r_V[c]], [rPS[4 + d]])
                                        for hl in range(2):
                                            self.act(rr[d][:, hl:hl + 1], PS[4 + d][:, hl * 144 + 128:hl * 144 + 129], AF.Abs, [rPS[4 + d]], [r_rr[d]])
                                        self.ts("dve", rr[d][:, 0:2], rr[d][:, 0:2], 1.0, None, ALU.max, None, [r_rr[d]], [r_rr[d]])
                                        P.op("dve", lambda e, d=d: e.reciprocal(out=rr[d][:, 2:4], in_=rr[d][:, 0:2]), [r_rr[d]], [r_rr[d]])
                                        for hl in range(2):
                                            self.stt("dve", HF[:, c, hl * 128:(hl + 1) * 128], PS[4 + d][:, hl * 144:hl * 144 + 128],
                                                     rr[d][:, 2 + hl:3 + hl], HF[:, c, hl * 128:(hl + 1) * 128], ALU.mult, ALU.add,
                                                     [rPS[4 + d], r_rr[d], r_HF[c]], [r_HF[c]])
                                    self.mm(PS[6 + d][:, 0:288], ks[d][:], Vh[:, c, :, :].rearrange("p h v -> p (h v)"), True, True,
                                            [r_ks[d], r_V[c]], [rPS[6 + d]])
                                    self.tt("dve", ctmp[d][0:64, :], Cf[d][0:64, :], PS[6 + d][0:64, 0:144], ALU.add, [r_Cf[d], rPS[6 + d]], [r_ctmp[d]])
                                    self.tt("dve", ctmp[d][64:128, :], Cf[d][64:128, :], PS[6 + d][64:128, 144:288], ALU.add,
                                            [r_Cf[d], rPS[6 + d]], [r_ctmp[d]])
                                    self.ts("dve", Cf[d][:], ctmp[d][:], EGC[:, c, d, hg:hg + 1], None, ALU.mult, None, [r_ctmp[d], r_EGC], [r_Cf[d]])
                                    self.cp("act", Cb[d][:], Cf[d][:], [r_Cf[d]], [r_Cb[d]])
                            if hg == 0:
                                self.tap("mHF", HF[:], [128, NL, 256], r_HF)
                            if cfg.get("ml_stop", 9) <= 4:
                                P.barrier()
                                return
                            slot2, rs2 = ring_next()
                            wo = slot2[:, 0:2048].rearrange("p (k n) -> p k n", k=8)
                            wout = slot2[:, 2048:4096].rearrange("p (k n) -> p k n", k=2)
                            P.dma("pool", wo, w3[:, :, 2048 + hg * 256:2048 + (hg + 1) * 256], writes=[rs2])
                            P.dma("pool", wout, w_out[0].rearrange("(k p) n -> p k n", p=128)[:, 2 * hg:2 * hg + 2, :], writes=[rs2])
                            for ti in range(NL):
                                u = ti % 2
                                bo = 4 * u
                                for k in range(8):
                                    self.mm(PS[bo][:, 0:256], hTm[:, k, ti * 128:(ti + 1) * 128], wo[:, k, :], k == 0, k == 7,
                                            [r_hTm[ti], rs2], [rPS[bo]])
                                self.act(og[:, u, :], PS[bo][:, 0:256], AF.Sigmoid, [rPS[bo]], [r_og[u]])
                                self.tt("dve", sq[u][:], HF[:, ti, :], HF[:, ti, :], ALU.mult, [r_HF[ti]], [r_sq[u]])
                                P.op("dve", lambda e, u=u: e.tensor_reduce(out=ss[u][:, 0:2], in_=sq[u][:].rearrange("p (h v) -> p h v", h=2),
                                                                          axis=AX.X, op=ALU.add), [r_sq[u]], [r_ss[u]])
                                self.act(ss[u][:, 2:4], ss[u][:, 0:2], AF.Sqrt, [r_ss[u], rK], [r_ss[u]], scale=1.0 / 128, bias=epsc[:, 0:1])
                                P.op("dve", lambda e, u=u: e.reciprocal(out=ss[u][:, 4:6], in_=ss[u][:, 2:4]), [r_ss[u]], [r_ss[u]])
                                self.tt("dve", t1[u][:].rearrange("p (h v) -> p h v", h=2), HF[:, ti, :].rearrange("p (h v) -> p h v", h=2),
                                        ss[u][:, 4:6].unsqueeze(2).to_broadcast([128, 2, 128]), ALU.mult, [r_HF[ti], r_ss[u]], [r_t1[u]])
                                self.tt("dve", t1[u][:], t1[u][:], NG[:, hg * 256:(hg + 1) * 256], ALU.mult, [r_t1[u], rT], [r_t1[u]])
                                self.tt("dve", ho[u][:], t1[u][:], og[:, u, :], ALU.mult, [r_t1[u], r_og[u]], [r_ho[u]])
                                psb = PS[bo + 1][:, :].bitcast(BF16)
                                for c2 in range(2):
                                    self.tr(psb[:, c2 * 128:(c2 + 1) * 128], ho[u][:, c2 * 128:(c2 + 1) * 128], identb[:], [r_ho[u], rK], [rPS[bo + 1]])
                                self.cp("act", hoT[u][:].rearrange("p a t -> p (a t)"), psb[:, 0:256], [rPS[bo + 1]], [r_hoT[u]])
                                for dh in range(2):
                                    for c2 in range(2):
                                        self.mm(PS[bo + 2 + dh][:, :], hoT[u][:, c2, :], wout[:, c2, dh * 512:(dh + 1) * 512], c2 == 0, c2 == 1,
                                                [r_hoT[u], rs2], [rPS[bo + 2 + dh]])
                                    self.tt("dve", y5[u][:, 0, :], PS[bo + 2 + dh][:, :], G1[:, dh * 512:(dh + 1) * 512], ALU.mult,
                                            [rPS[bo + 2 + dh], rT], [r_y5[u]])
                                    self.tt("dve", X[:, ti, dh * 512:(dh + 1) * 512], X[:, ti, dh * 512:(dh + 1) * 512], y5[u][:, 0, :], ALU.add,
                                            [r_y5[u], rX[ti]], [rX[ti]])
                        P.barrier()

            if cfg.get("do_mlstm", True):
                mlstm()
            self.tap("xmix1", X[:], [128, NT, D], rX)
            if cfg.get("do_moe1", True):
                moe(1, False)

            with ExitStack() as s5:
                FV = self.sb(s5, "fv", [128, D], F32)
                r_FV = P.res("fv")
                fj = self.sb(s5, "fj", [128, D], BF16)
                r_fj = P.res("fj")
                P.dma("sp", FV[:], final_norm[0:1, :].partition_broadcast(128), writes=[r_FV])
                lat = list(range(NL))
                for ti in lat:
                    self.act(fj[:], X[:, ti, :], AF.Square, [rX[ti]], [r_stat, r_fj], accum_out=stat[:, 0, ti:ti + 1])
                self.act(stat[:, 1, :], stat[:, 0, :], AF.Sqrt, [r_stat, rK], [r_stat], scale=1.0 / D, bias=epsc[:, 0:1])
                P.op("dve", lambda e: e.reciprocal(out=stat[:, 2, :], in_=stat[:, 1, :]), [r_stat], [r_stat])
                ov = out_d.rearrange("(t p) d -> p t d", p=128)
                for ti in lat:
                    self.stt("dve", X[:, ti, :], X[:, ti, :], stat[:, 2, ti:ti + 1], FV[:], ALU.mult, ALU.mult,
                             [rX[ti], r_stat, r_FV], [rX[ti]])
                    P.dma("sp", ov[:, ti, :], X[:, ti, :], reads=[rX[ti]])
                P.emit(st)
        return nc


_CACHE = {}


def _layout_inputs(inputs, b):
    m = {}
    m["x"] = np.ascontiguousarray(inputs["x"][b])
    m["ctx"] = np.ascontiguousarray(inputs["ctx"][b])
    cc = np.stack([inputs["c"][b], inputs["c_ctx"]], axis=-1)
    m["cc"] = np.ascontiguousarray(cc.reshape(8, 128, 2).transpose(1, 0, 2))
    for k in ("ada_w", "ada_b", "norm_mix", "norm_ffn", "pool_w", "pool_scale", "mlstm_w_in", "mlstm_b_gates",
              "mlstm_norm", "mlstm_w_out", "moe_router", "moe_w_gate", "moe_w_up", "moe_w_down"):
        m[k] = inputs[k]
    m["final_norm"] = inputs["final_norm"].reshape(1, D)
    m.update(host_constants())
    return m


def kernel(**inputs):
    inputs = {k: np.asarray(v) for k, v in inputs.items()}
    n = 8
    bld = Builder()
    nc = bld.build()
    in_maps = [_layout_inputs(inputs, b) for b in range(n)]
    res = run_bass_kernel_spmd(nc, in_maps, core_ids=list(range(n)))
    return np.stack([r["out"] for r in res.results], axis=0).astype(np.float32)
```

```python
import numpy as np
import ml_dtypes
from contextlib import ExitStack
import concourse.bass as bass
import concourse.mybir as mybir
from concourse.bass_utils import run_bass_kernel_spmd

F32 = mybir.dt.float32
BF16 = mybir.dt.bfloat16
ALU = mybir.AluOpType
AF = mybir.ActivationFunctionType
AX = mybir.AxisListType

D = 1024
SEQ = 2048
CTX = 256
NT = 18
NL = 16
E = 16
FH = 2048
POOL_WINDOWS = (2, 4, 8, 16)
EPS = 1e-6

SAME_ENGINE_SYNC = True
ENGS = ("pe", "dve", "act", "pool", "sp")


class Res:
    __slots__ = ("name", "w", "rs", "const")

    def __init__(self, name, const=False):
        self.name = name
        self.w = None
        self.rs = []
        self.const = const


class Op:
    __slots__ = ("eng", "fn", "deps", "signal", "is_dma", "lane", "cum", "sig_idx", "lane_prev")

    def __init__(self, eng, fn, is_dma=False):
        self.eng = eng
        self.fn = fn
        self.deps = set()
        self.signal = False
        self.is_dma = is_dma
        self.lane = None
        self.cum = 0
        self.sig_idx = 0
        self.lane_prev = 0


class Prog:
    def __init__(self, nc, n_lanes=8):
        self.nc = nc
        self.ops = {e: [] for e in ENGS}
        self.last = {e: None for e in ENGS}
        self.n_lanes = n_lanes
        self.lane_rr = {"sp": 0, "pool": 0, "act": 0}
        self.lane_cum = {}
        self.lane_last = {}
        self.pending = {e: set() for e in ENGS}
        self.nres = 0

    def res(self, name=None, const=False):
        self.nres += 1
        return Res(name or f"r{self.nres}", const)

    def _track(self, op, reads, writes):
        for r in reads:
            if r.w is not None:
                op.deps.add(r.w)
            if not r.const:
                r.rs.append(op)
        for r in writes:
            if r.w is not None:
                op.deps.add(r.w)
            for o in r.rs:
                op.deps.add(o)
            r.w = op
            r.rs = []
        if self.pending[op.eng]:
            op.deps |= self.pending[op.eng]
            self.pending[op.eng] = set()
        op.deps.discard(op)

    def op(self, eng, fn, reads=(), writes=()):
        o = Op(eng, fn)
        self._track(o, reads, writes)
        self.ops[eng].append(o)
        self.last[eng] = o
        return o

    def dma(self, q, out, in_, reads=(), writes=(), **kw):
        o = Op(q, None, is_dma=True)

        def fn(e, out=out, in_=in_, kw=kw):
            return e.dma_start(out=out, in_=in_, **kw)
        o.fn = fn
        lane = (q, self.lane_rr[q] % self.n_lanes)
        self.lane_rr[q] += 1
        o.lane = lane
        o.lane_prev = self.lane_cum.get(lane, 0)
        o.cum = o.lane_prev + 1
        self.lane_cum[lane] = o.cum
        self.lane_last[lane] = o
        self._track(o, reads, writes)
        self.ops[q].append(o)
        self.last[q] = o
        return o

    def barrier(self):
        deps = set(o for o in self.last.values() if o is not None)
        deps |= set(self.lane_last.values())
        for e in ENGS:
            self.pending[e] |= deps

    def emit(self, stack):
        nc = self.nc
        for e in ENGS:
            for o in self.ops[e]:
                for d in o.deps:
                    if d.is_dma:
                        continue
                    if d.eng != o.eng or (SAME_ENGINE_SYNC and o.eng != "pe"):
                        d.signal = True
        sems = {}
        for e in ENGS:
            sems[e] = stack.enter_context(nc.semaphore(f"s_{e}"))
            k = 0
            for o in self.ops[e]:
                if o.signal and not o.is_dma:
                    k += 1
                    o.sig_idx = k
        lane_sems = {}
        for lane in self.lane_cum:
            lane_sems[lane] = stack.enter_context(nc.semaphore(f"l_{lane[0]}{lane[1]}"))

        def run(ename, eng):
            waited = {}
            for o in self.ops[ename]:
                need = {}
                for d in o.deps:
                    if d.is_dma:
                        key = ("L", d.lane)
                        val = 16 * d.cum
                    else:
                        if d.eng == ename and (ename == "pe" or not SAME_ENGINE_SYNC):
                            continue
                        key = ("E", d.eng)
                        val = d.sig_idx
                    if val > need.get(key, 0):
                        need[key] = val
                if o.is_dma and o.lane_prev > 0:
                    key = ("L", o.lane)
                    val = 16 * o.lane_prev
                    if val > need.get(key, 0):
                        need[key] = val
                for key, val in need.items():
                    if waited.get(key, 0) >= val:
                        continue
                    waited[key] = val
                    s = lane_sems[key[1]] if key[0] == "L" else sems[key[1]]
                    eng.wait_ge(s, val)
                ins = o.fn(eng)
                if o.is_dma:
                    ins.then_inc(lane_sems[o.lane], 16)
                elif o.signal:
                    ins.then_inc(sems[ename], 1)
            for lane, cum in self.lane_cum.items():
                if lane[0] == ename and waited.get(("L", lane), 0) < 16 * cum:
                    eng.wait_ge(lane_sems[lane], 16 * cum)

        with nc.Block() as block:
            @block.sync
            def _(e):
                run("sp", e)

            @block.tensor
            def _(e):
                run("pe", e)

            @block.vector
            def _(e):
                run("dve", e)

            @block.scalar
            def _(e):
                run("act", e)

            @block.gpsimd
            def _(e):
                run("pool", e)


def _pool_constants():
    blocks = {}
    blk_list = []
    lat_map = {}
    ctx_map = {}
    invcnt = np.zeros((128, NT, 4), np.float32)

    def add(mat):
        key = mat.tobytes()
        if key not in blocks:
            blocks[key] = len(blk_list)
            blk_list.append(mat)
        return blocks[key]

    r = np.arange(32)
    wv = np.arange(64)
    for j, w in enumerate(POOL_WINDOWS):
        h = w // 2
        Br = ((r[None, :] >= r[:, None] - h) & (r[None, :] < r[:, None] + h)).astype(np.float32)
        Bw = ((wv[None, :] >= wv[:, None] - h) & (wv[None, :] < wv[:, None] + h)).astype(np.float32)
        M = np.kron(Br, Bw)
        cnt = M.sum(1)
        M = M - np.diag(cnt)
        invcnt[:, :NL, j] = (1.0 / cnt).reshape(NL, 128).T
        for io in range(NL):
            for ii in range(NL):
                b = M[io * 128:(io + 1) * 128, ii * 128:(ii + 1) * 128]
                if np.any(b):
                    lat_map[(j, io, ii)] = add(np.ascontiguousarray(b.T))
        t = np.arange(CTX)
        Mc = ((t[None, :] >= t[:, None] - h) & (t[None, :] < t[:, None] + h)).astype(np.float32)
        cc = Mc.sum(1)
        Mc = Mc - np.diag(cc)
        invcnt[:, NL:, j] = (1.0 / cc).reshape(2, 128).T
        for io in range(2):
            for ii in range(2):
                b = Mc[io * 128:(io + 1) * 128, ii * 128:(ii + 1) * 128]
                if np.any(b):
                    ctx_map[(j, io, ii)] = add(np.ascontiguousarray(b.T))
    blks = np.stack(blk_list).astype(ml_dtypes.bfloat16)
    assert np.all(blks.astype(np.float32) == np.stack(blk_list))
    return blks, lat_map, ctx_map, invcnt


_PC = None


def pool_constants():
    global _PC
    if _PC is None:
        _PC = _pool_constants()
    return _PC


def host_constants():
    blks, lat_map, ctx_map, invcnt = pool_constants()
    c = {}
    c["k_pblk"] = np.ascontiguousarray(blks.transpose(1, 0, 2))
    c["k_invcnt"] = invcnt
    c["k_identb"] = np.eye(128, dtype=np.float32).astype(ml_dtypes.bfloat16)
    c["k_identf"] = np.eye(128, dtype=np.float32)
    c["k_iota"] = np.tile(np.arange(256, dtype=np.float32)[None, :], (128, 1))
    ip = np.zeros((128, 4), np.float32)
    ip[:, 0] = np.arange(128)
    ip[:, 1] = np.arange(128) + 128
    c["k_iotap"] = ip
    oh = np.zeros((16, 16, 128), np.float32)
    for e in range(16):
        oh[e, e, :] = 1.0
    c["k_onehot"] = oh.astype(ml_dtypes.bfloat16)
    c["k_ones"] = np.ones((128, 2048), np.float32)
    tri = np.zeros((128, 3, 128), np.float32)
    tri[:, 0, :] = np.triu(np.ones((128, 128), np.float32))
    tri[:, 1, :] = np.tril(np.ones((128, 128), np.float32))
    tri[:, 2, :] = 1.0
    c["k_tri"] = tri
    c["k_mask2"] = np.ascontiguousarray(np.concatenate([tri[:, 0:2, :], tri[:, 0:2, :]], axis=2))
    return c


class Builder:
    def __init__(self, cfg=None):
        self.cfg = cfg or {}
        self.nc = bass.Bass("TRN2", target_bir_lowering=False)
        self.P = Prog(self.nc)
        self.taps = {}

    def sb(self, st, name, shape, dt):
        self._nsb = getattr(self, "_nsb", 0) + 1
        return st.enter_context(self.nc.sbuf_tensor(f"{name}_{self._nsb}", list(shape), dt))

    def din(self, name, shape, dt=F32):
        return self.nc.dram_tensor(name, list(shape), dt, kind="ExternalInput").ap()

    def mm(self, out, lhsT, rhs, start, stop, reads, writes):
        self.P.op("pe", lambda e: e.matmul(out, lhsT=lhsT, rhs=rhs, start=start, stop=stop), reads, writes)

    def tr(self, out, in_, ident, reads, writes):
        self.P.op("pe", lambda e: e.transpose(out, in_, ident), reads, writes)

    def act(self, out, in_, func, reads, writes, **kw):
        self.P.op("act", lambda e: e.activation(out=out, in_=in_, func=func, **kw), reads, writes)

    def tt(self, eng, out, in0, in1, op, reads, writes):
        self.P.op(eng, lambda e: e.tensor_tensor(out=out, in0=in0, in1=in1, op=op), reads, writes)

    def ts(self, eng, out, in0, s1, s2, op0, op1, reads, writes):
        if s2 is None:
            self.P.op(eng, lambda e: e.tensor_scalar(out=out, in0=in0, scalar1=s1, scalar2=None, op0=op0), reads, writes)
        else:
            self.P.op(eng, lambda e: e.tensor_scalar(out=out, in0=in0, scalar1=s1, scalar2=s2, op0=op0, op1=op1), reads, writes)

    def stt(self, eng, out, in0, scalar, in1, op0, op1, reads, writes):
        self.P.op(eng, lambda e: e.scalar_tensor_tensor(out=out, in0=in0, scalar=scalar, in1=in1, op0=op0, op1=op1), reads, writes)

    def cp(self, eng, out, in_, reads, writes):
        if eng == "act":
            self.P.op("act", lambda e: e.copy(out=out, in_=in_), reads, writes)
        else:
            self.P.op(eng, lambda e: e.tensor_copy(out=out, in_=in_), reads, writes)

    def tap(self, name, sb_ap, shape, reads, dt=F32):
        if name not in self.cfg.get("taps", ()):
            return
        d = self.nc.dram_tensor("tap_" + name, list(shape), dt, kind="ExternalOutput").ap()
        self.P.dma("sp", d, sb_ap, reads=reads)
        self.taps[name] = "tap_" + name

    def build(self):
        nc, P, cfg = self.nc, self.P, self.cfg
        blks, lat_map, ctx_map, _ = pool_constants()
        NB = blks.shape[0]
        x_d = self.din("x", [SEQ, D])
        ctx_d = self.din("ctx", [CTX, D])
        cc_d = self.din("cc", [128, 8, 2])
        ada_w = self.din("ada_w", [2, D, 6 * D])
        ada_b = self.din("ada_b", [2, 6 * D])
        norm_mix = self.din("norm_mix", [2, D])
        norm_ffn = self.din("norm_ffn", [2, D])
        pool_w = self.din("pool_w", [1, 4, 256, 256])
        pool_scale = self.din("pool_scale", [1, D])
        w_in = self.din("mlstm_w_in", [1, D, 3104])
        b_gates = self.din("mlstm_b_gates", [1, 32])
        m_norm = self.din("mlstm_norm", [1, D])
        w_out = self.din("mlstm_w_out", [1, D, D])
        router = self.din("moe_router", [2, D, E])
        EW = cfg.get("ew", E)
        w_gate = self.din("moe_w_gate", [2, EW, D, FH])
        w_up = self.din("moe_w_up", [2, EW, D, FH])
        w_down = self.din("moe_w_down", [2, EW, FH, D])
        final_norm = self.din("final_norm", [1, D])
        k_pblk = self.din("k_pblk", [128, NB, 128], BF16)
        k_invcnt = self.din("k_invcnt", [128, NT, 4])
        k_identb = self.din("k_identb", [128, 128], BF16)
        k_identf = self.din("k_identf", [128, 128])
        k_iota = self.din("k_iota", [128, 256])
        k_iotap = self.din("k_iotap", [128, 4])
        k_onehot = self.din("k_onehot", [16, 16, 128], BF16)
        k_ones = self.din("k_ones", [128, 2048])
        k_tri = self.din("k_tri", [128, 3, 128])
        k_mask2 = self.din("k_mask2", [128, 2, 256])
        out_d = nc.dram_tensor("out", [SEQ, D], F32, kind="ExternalOutput").ap()
        modv = nc.dram_tensor("modv", [2, 6, 2, D], F32).ap()

        with ExitStack() as st:
            X = self.sb(st, "X", [128, NT, D], F32)
            rX = [P.res(f"X{i}") for i in range(NT)]
            identb = self.sb(st, "identb", [128, 128], BF16)
            identf = self.sb(st, "identf", [128, 128], F32)
            iota = self.sb(st, "iota", [128, 256], F32)
            iotap = self.sb(st, "iotap", [128, 4], F32)
            epsc = self.sb(st, "epsc", [128, 1], F32)
            stat = self.sb(st, "stat", [128, 4, NT], F32)
            rK = P.res("consts", const=True)
            r_stat = P.res("stat")
            for t_, d_ in ((identb, k_identb), (identf, k_identf), (iota, k_iota), (iotap, k_iotap)):
                P.dma("sp", t_[:], d_, writes=[rK])
            P.op("dve", lambda e: e.memset(epsc[:], EPS), writes=[rK])
            PS = [st.enter_context(nc.psum_tensor(f"ps{i}", [128, 512], F32)) for i in range(8)]
            rPS = [P.res(f"ps{i}") for i in range(8)]
            NSLOT = cfg.get("nslot", 4)
            RING = [self.sb(st, f"ring{i}", [128, 4096], BF16) for i in range(NSLOT)]
            rRING = [P.res(f"ring{i}") for i in range(NSLOT)]
            self.ring_i = 0

            def ring_next():
                i = self.ring_i % NSLOT
                self.ring_i += 1
                return RING[i], rRING[i]

            xv = x_d.rearrange("(t p) d -> p t d", p=128)
            cv = ctx_d.rearrange("(t p) d -> p t d", p=128)
            for ti in range(NL):
                P.dma("sp", X[:, ti, :], xv[:, ti, :], writes=[rX[ti]])
            for ti in range(2):
                P.dma("sp", X[:, NL + ti, :], cv[:, ti, :], writes=[rX[NL + ti]])

            with ExitStack() as s0:
                cc_t = self.sb(s0, "cc_t", [128, 8, 2], F32)
                scb = self.sb(s0, "scb", [128, 8, 2], BF16)
                modrow = self.sb(s0, "modrow", [2, 6 * D], F32)
                adab = self.sb(s0, "adab", [2, 6 * D], F32)
                vec = self.sb(s0, "vec", [2, 6, D], F32)
                grow = self.sb(s0, "grow", [2, 3, D], F32)
                r_cc, r_scb, r_mod, r_adab, r_vec, r_grow = [P.res(n) for n in "cc scb mod adab vec grow".split()]
                P.dma("sp", cc_t[:], cc_d, writes=[r_cc])
                self.act(scb[:], cc_t[:], AF.Silu, [r_cc], [r_scb])
                r_modv = P.res("modv")
                self.r_modv = r_modv
                for i in range(2):
                    for s in range(2):
                        P.dma("sp", adab[s:s + 1, :], ada_b[i:i + 1, :], writes=[r_adab])
                        P.dma("sp", grow[s:s + 1, 0, :], norm_mix[i:i + 1, :], writes=[r_grow])
                        P.dma("sp", grow[s:s + 1, 1, :], norm_ffn[i:i + 1, :], writes=[r_grow])
                        if i == 0:
                            P.dma("sp", grow[s:s + 1, 2, :], pool_scale[0:1, :], writes=[r_grow])
                    awv = ada_w[i].rearrange("(k p) n -> p k n", p=128)
                    for nb in range(12):
                        slot, rs = ring_next()
                        sl3 = slot[:].rearrange("p (k n) -> p k n", k=8)
                        P.dma("pool", sl3, awv[:, :, nb * 512:(nb + 1) * 512], writes=[rs])
                        pb = nb % 2
                        for k in range(8):
                            self.mm(PS[pb][0:2, :], scb[:, k, :], sl3[:, k, :], k == 0, k == 7, [r_scb, rs], [rPS[pb]])
                        self.tt("dve", modrow[:, nb * 512:(nb + 1) * 512], PS[pb][0:2, :], adab[:, nb * 512:(nb + 1) * 512],
                                ALU.add, [rPS[pb], r_adab], [r_mod])
                    self.stt("dve", vec[:, 0, :], modrow[:, D:2 * D], 1.0, grow[:, 0, :], ALU.add, ALU.mult, [r_mod, r_grow], [r_vec])
                    self.cp("dve", vec[:, 1, :], modrow[:, 0:D], [r_mod], [r_vec])
                    if i == 0:
                        self.tt("dve", vec[:, 2, :], modrow[:, 2 * D:3 * D], grow[:, 2, :], ALU.mult, [r_mod, r_grow], [r_vec])
                    else:
                        self.cp("dve", vec[:, 2, :], modrow[:, 2 * D:3 * D], [r_mod], [r_vec])
                    self.stt("dve", vec[:, 3, :], modrow[:, 4 * D:5 * D], 1.0, grow[:, 1, :], ALU.add, ALU.mult, [r_mod, r_grow], [r_vec])
                    self.cp("dve", vec[:, 4, :], modrow[:, 3 * D:4 * D], [r_mod], [r_vec])
                    self.cp("dve", vec[:, 5, :], modrow[:, 5 * D:6 * D], [r_mod], [r_vec])
                    P.dma("sp", modv[i].rearrange("j s d -> s j d"), vec[:], reads=[r_vec], writes=[r_modv])
                P.barrier()

            def load_vec(tile, rtile, src_row):
                P.dma("sp", tile[:], src_row.partition_broadcast(128), reads=[self.r_modv], writes=[rtile])

            def rstd_for(tiles, Hjunk):
                for ti in tiles:
                    self.act(Hjunk(ti), X[:, ti, :], AF.Square, [rX[ti]], [r_stat], accum_out=stat[:, 0, ti:ti + 1])
                self.act(stat[:, 1, :], stat[:, 0, :], AF.Sqrt, [r_stat, rK], [r_stat], scale=1.0 / D, bias=epsc[:, 0:1])
                P.op("dve", lambda e: e.reciprocal(out=stat[:, 2, :], in_=stat[:, 1, :]), [r_stat], [r_stat])

            all_tiles = list(range(NT))

            with ExitStack() as s1:
                H = self.sb(s1, "H", [128, NT, D], BF16)
                rH = [P.res(f"H{i}") for i in range(NT)]
                MV = [[self.sb(s1, f"mv{s}{j}", [128, D], F32) for j in range(2)] for s in range(2)]
                rMV = [[P.res(f"mv{s}{j}") for j in range(2)] for s in range(2)]
                tmpf = self.sb(s1, "tmpf", [128, D], F32)
                r_tmpf = P.res("tmpf")
                pblk = self.sb(s1, "pblk", [128, NB, 128], BF16)
                invc = self.sb(s1, "invc", [128, NT, 4], F32)
                wp = self.sb(s1, "wp", [128, 4, 2, 256], BF16)
                ut = self.sb(s1, "ut", [128, 2, 2, 128], BF16)
                ptmp = self.sb(s1, "ptmp", [128, 2, 256], F32)
                r_pk = P.res("poolconst")
                r_ut = [P.res("ut0"), P.res("ut1")]
                r_pt = [P.res("pt0"), P.res("pt1")]
                P.dma("sp", pblk[:], k_pblk, writes=[r_pk])
                P.dma("sp", invc[:], k_invcnt, writes=[r_pk])
                P.dma("pool", wp[:], pool_w[0].rearrange("g (k p) d -> p g k d", p=128), writes=[r_pk])
                for s in range(2):
                    load_vec(MV[s][0], rMV[s][0], modv[0, 0, s:s + 1, :])
                    load_vec(MV[s][1], rMV[s][1], modv[0, 1, s:s + 1, :])
                for ti in all_tiles:
                    self.act(H[:, ti, :], X[:, ti, :], AF.Square, [rX[ti]], [r_stat, rH[ti]], accum_out=stat[:, 0, ti:ti + 1])
                self.act(stat[:, 1, :], stat[:, 0, :], AF.Sqrt, [r_stat, rK], [r_stat], scale=1.0 / D, bias=epsc[:, 0:1])
                P.op("dve", lambda e: e.reciprocal(out=stat[:, 2, :], in_=stat[:, 1, :]), [r_stat], [r_stat])
                for ti in all_tiles:
                    s = 0 if ti < NL else 1
                    self.stt("dve", tmpf[:], X[:, ti, :], stat[:, 2, ti:ti + 1], MV[s][0][:], ALU.mult, ALU.mult,
                             [rX[ti], r_stat, rMV[s][0]], [r_tmpf])
                    self.tt("dve", H[:, ti, :], tmpf[:], MV[s][1][:], ALU.add, [r_tmpf, rMV[s][1]], [rH[ti]])
                self.tap("h0", H[:], [128, NT, D], rH, BF16)
                for s in range(2):
                    load_vec(MV[s][0], rMV[s][0], modv[0, 2, s:s + 1, :])
                cnt = 0
                for j in range(4):
                    for io in range(NT):
                        s = 0 if io < NL else 1
                        if s == 0:
                            nb_ = [(ii, lat_map[(j, io, ii)]) for ii in range(NL) if (j, io, ii) in lat_map]
                        else:
                            nb_ = [(NL + ii, ctx_map[(j, io - NL, ii)]) for ii in range(2) if (j, io - NL, ii) in ctx_map]
                        b1 = cnt % 2
                        b2 = 2 + cnt % 2
                        u = cnt % 2
                        cnt += 1
                        for c in range(2):
                            ch = 2 * j + c
                            for n, (ii, bid) in enumerate(nb_):
                                self.mm(PS[b1][:, c * 128:(c + 1) * 128], H[:, ii, ch * 128:(ch + 1) * 128], pblk[:, bid, :],
                                        n == 0, n == len(nb_) - 1, [rH[ii], r_pk], [rPS[b1]])
                        self.cp("act", ut[:, u, :, :].rearrange("p c t -> p (c t)"), PS[b1][:, 0:256], [rPS[b1]], [r_ut[u]])
                        for c in range(2):
                            self.mm(PS[b2][:, 0:256], ut[:, u, c, :], wp[:, j, c, :], c == 0, c == 1, [r_ut[u], r_pk], [rPS[b2]])
                        self.stt("dve", ptmp[:, u, :], PS[b2][:, 0:256], invc[:, io, j:j + 1], MV[s][0][:, j * 256:(j + 1) * 256],
                                 ALU.mult, ALU.mult, [rPS[b2], r_pk, rMV[s][0]], [r_pt[u]])
                        self.tt("dve", X[:, io, j * 256:(j + 1) * 256], X[:, io, j * 256:(j + 1) * 256], ptmp[:, u, :], ALU.add,
                                [r_pt[u], rX[io]], [rX[io]])
                P.barrier()
            self.tap("xmix0", X[:], [128, NT, D], rX)

            def moe(layer, with_ctx):
                tiles = all_tiles if with_ctx else list(range(NL))
                nt_ = len(tiles)
                n_exp = cfg.get("n_exp", E)
                tot = SEQ + (CTX if with_ctx else 0)
                with ExitStack() as s2:
                    H = self.sb(s2, "Hm", [128, NT, D], BF16)
                    rH = [P.res(f"Hm{i}") for i in range(NT)]
                    slotb = self.sb(s2, "slotb", [16, SEQ + CTX], BF16)
                    affTb = self.sb(s2, "affTb", [16, SEQ + CTX], BF16)
                    slotT = self.sb(s2, "slotT", [128, NT, E], F32)
                    onehot = self.sb(s2, "onehot", [16, 16, 128], BF16)
                    P.dma("sp", onehot[:], k_onehot, writes=[rK])
                    r_slotb, r_affTb, r_slotT = [P.res(n) for n in "slotb affTb slotT".split()]
                    with ExitStack() as s3:
                        MV = [[self.sb(s3, f"mw{s}{j}", [128, D], F32) for j in range(2)] for s in range(2)]
                        rMV = [[P.res(f"mw{s}{j}") for j in range(2)] for s in range(2)]
                        tmpf = self.sb(s3, "tmpg", [128, D], F32)
                        r_tmpf = P.res("tmpg")
                        wr = self.sb(s3, "wr", [128, 8, E], F32)
                        hT = self.sb(s3, "hT", [128, 8, 128], F32)
                        logit = self.sb(s3, "logit", [128, NT, E], F32)
                        aff = self.sb(s3, "aff", [128, NT, E], F32)
                        sm = self.sb(s3, "sm", [128, 4, NT], F32)
                        affT = self.sb(s3, "affT", [16, SEQ + CTX], F32)
                        slotf = self.sb(s3, "slotf", [16, SEQ + CTX], F32)
                        mx8 = self.sb(s3, "mx8", [16, 8], F32)
                        r_wr, r_hT, r_logit, r_aff, r_sm, r_affT, r_mx8, r_slotf = [
                            P.res(n) for n in "wr hT logit aff sm affT mx8 slotf".split()]
                        P.dma("sp", wr[:], router[layer].rearrange("(k p) e -> p k e", p=128), writes=[r_wr])
                        for s in range(2 if with_ctx else 1):
                            load_vec(MV[s][0], rMV[s][0], modv[layer, 3, s:s + 1, :])
                            load_vec(MV[s][1], rMV[s][1], modv[layer, 4, s:s + 1, :])
                        for ti in tiles:
                            self.act(H[:, ti, :], X[:, ti, :], AF.Square, [rX[ti]], [r_stat, rH[ti]], accum_out=stat[:, 0, ti:ti + 1])
                        self.act(stat[:, 1, :], stat[:, 0, :], AF.Sqrt, [r_stat, rK], [r_stat], scale=1.0 / D, bias=epsc[:, 0:1])
                        P.op("dve", lambda e: e.reciprocal(out=stat[:, 2, :], in_=stat[:, 1, :]), [r_stat], [r_stat])
                        for ti in tiles:
                            s = 0 if ti < NL else 1
                            self.stt("dve", tmpf[:], X[:, ti, :], stat[:, 2, ti:ti + 1], MV[s][0][:], ALU.mult, ALU.mult,
                                     [rX[ti], r_stat, rMV[s][0]], [r_tmpf])
                            self.tt("dve", tmpf[:], tmpf[:], MV[s][1][:], ALU.add, [r_tmpf, rMV[s][1]], [r_tmpf])
                            self.cp("act", H[:, ti, :], tmpf[:], [r_tmpf], [rH[ti]])
                            for k in range(8):
                                b = k // 4
                                self.tr(PS[b][:, (k % 4) * 128:(k % 4 + 1) * 128], tmpf[:, k * 128:(k + 1) * 128], identf[:],
                                        [r_tmpf, rK], [rPS[b]])
                            for b in range(2):
                                self.cp("act", hT[:, b * 4:(b + 1) * 4, :].rearrange("p k t -> p (k t)"), PS[b][:, :], [rPS[b]], [r_hT])
                            for k in range(8):
                                self.mm(PS[2][:, 0:E], hT[:, k, :], wr[:, k, :], k == 0, k == 7, [r_hT, r_wr], [rPS[2]])
                            self.cp("dve", logit[:, ti, :], PS[2][:, 0:E], [rPS[2]], [r_logit])
                        P.op("dve", lambda e: e.tensor_reduce(out=sm[:, 0, 0:nt_], in_=logit[:, 0:nt_, :], axis=AX.X, op=ALU.max),
                             [r_logit], [r_sm])
                        self.tt("dve", aff[:, 0:nt_, :], logit[:, 0:nt_, :], sm[:, 0, 0:nt_].unsqueeze(2).to_broadcast([128, nt_, E]),
                                ALU.subtract, [r_logit, r_sm], [r_aff])
                        self.act(aff[:, 0:nt_, :], aff[:, 0:nt_, :], AF.Exp, [r_aff], [r_aff])
                        P.op("dve", lambda e: e.tensor_reduce(out=sm[:, 1, 0:nt_], in_=aff[:, 0:nt_, :], axis=AX.X, op=ALU.add),
                             [r_aff], [r_sm])
                        P.op("dve", lambda e: e.reciprocal(out=sm[:, 2, 0:nt_], in_=sm[:, 1, 0:nt_]), [r_sm], [r_sm])
                        self.tt("dve", aff[:, 0:nt_, :], aff[:, 0:nt_, :], sm[:, 2, 0:nt_].unsqueeze(2).to_broadcast([128, nt_, E]),
                                ALU.mult, [r_aff, r_sm], [r_aff])
                        self.tap(f"aff{layer}", aff[:], [128, NT, E], [r_aff])
                        for g in range((nt_ + 3) // 4):
                            tl = tiles[g * 4:(g + 1) * 4]
                            for n, ti in enumerate(tl):
                                self.tr(PS[3][0:16, n * 128:(n + 1) * 128], aff[:, ti, :], identf[:], [r_aff, rK], [rPS[3]])
                            w_ = len(tl) * 128
                            self.cp("act", affT[:, g * 512:g * 512 + w_], PS[3][0:16, 0:w_], [rPS[3]], [r_affT])
                        self.cp("act", affTb[:, 0:tot], affT[:, 0:tot], [r_affT], [r_affTb])
                        groups = [(0, SEQ, 256)] + ([(SEQ, CTX, 32)] if with_ctx else [])
                        for (o0, n0, k0) in groups:
                            for it in range(k0 // 8):
                                P.op("dve", lambda e, o0=o0, n0=n0: e.max(out=mx8[:], in_=affT[:, o0:o0 + n0]), [r_affT], [r_mx8])
                                P.op("dve", lambda e, o0=o0, n0=n0: e.match_replace(out=affT[:, o0:o0 + n0], in_to_replace=mx8[:],
                                                                                   in_values=affT[:, o0:o0 + n0], imm_value=0.0),
                                     [r_affT, r_mx8], [r_affT])
                        self.ts("dve", affT[:, 0:tot], affT[:, 0:tot], 0.0, None, ALU.is_equal, None, [r_affT], [r_affT])
                        for (o0, n0, k0) in groups:
                            P.op("dve", lambda e, o0=o0, n0=n0: e.tensor_tensor_scan(out=slotf[:, o0:o0 + n0], data0=affT[:, o0:o0 + n0],
                                                                                     data1=affT[:, o0:o0 + n0], initial=0.0,
                                                                                     op0=ALU.add, op1=ALU.max),
                                 [r_affT], [r_slotf])
                        self.tt("dve", slotf[:, 0:tot], slotf[:, 0:tot], affT[:, 0:tot], ALU.mult, [r_slotf, r_affT], [r_slotf])
                        self.ts("dve", slotf[:, 0:tot], slotf[:, 0:tot], -1.0, None, ALU.add, None, [r_slotf], [r_slotf])
                        self.cp("dve", slotb[:, 0:tot], slotf[:, 0:tot], [r_slotf], [r_slotb])
                        for g in range((nt_ + 3) // 4):
                            tl = tiles[g * 4:(g + 1) * 4]
                            for n, ti in enumerate(tl):
                                self.tr(PS[3][:, n * 16:(n + 1) * 16], slotf[:, ti * 128:(ti + 1) * 128], identf[0:16, 0:16],
                                        [r_slotf, rK], [rPS[3]])
                            self.cp("act", slotT[:, g * 4:g * 4 + len(tl), :].rearrange("p t e -> p (t e)"), PS[3][:, 0:len(tl) * 16],
                                    [rPS[3]], [r_slotT])
                        self.tap(f"slotT{layer}", slotT[:], [128, NT, E], [r_slotT])
                        P.barrier()

                    with ExitStack() as s4:
                        GV = [self.sb(s4, f"gv{s}", [128, D], F32) for s in range(2)]
                        rGV = [P.res(f"gv{s}") for s in range(2)]
                        selL = self.sb(s4, "selL", [128, NL, 256], BF16)
                        selC = self.sb(s4, "selC", [128, 2, 32], BF16)
                        sgt = self.sb(s4, "sgt", [128, 2, SEQ], BF16)
                        sgtC = self.sb(s4, "sgtC", [128, CTX], BF16)
                        abro = [self.sb(s4, f"abro{i}", [128, 512], F32) for i in range(2)]
                        xst = [self.sb(s4, f"xst{i}", [128, 8, 288 if with_ctx else 256], BF16) for i in range(2)]
                        hid = self.sb(s4, "hid", [128, 2, 4, 288], BF16)
                        sg = self.sb(s4, "sg", [128, 2, 288], F32)
                        yb = self.sb(s4, "yb", [128, 2, D], BF16)
                        ybC = self.sb(s4, "ybC", [128, D], BF16)
                        r_selL, r_selC, r_sgtC = [P.res(n) for n in "selL selC sgtC".split()]
                        r_sgt = [[P.res(f"sgt{h_}{t_}") for t_ in range(4)] for h_ in range(2)]
                        r_yb = [[P.res(f"yb{h_}{d_}") for d_ in range(2)] for h_ in range(2)]
                        r_ybC = [P.res("ybC0"), P.res("ybC1")]
                        r_abro = [P.res("abro0"), P.res("abro1")]
                        r_xst = [[P.res(f"xst{i_}{k_}") for k_ in range(8)] for i_ in range(2)]
                        r_sg = [P.res("sg0"), P.res("sg1")]
                        r_hid = [[P.res(f"hid{h_}{c_}") for c_ in range(4)] for h_ in range(2)]
                        for s in range(2 if with_ctx else 1):
                            load_vec(GV[s], rGV[s], modv[layer, 5, s:s + 1, :])
                        S = 288 if with_ctx else 256
                        wgv = w_gate[layer]
                        wuv = w_up[layer]
                        wdv = w_down[layer]
                        halves = [(0, 128), (1, 128)] + ([(2, 32)] if with_ctx else [])
                        def sel_gather(ex):
                            xs = xst[ex % len(xst)]
                            rxs = r_xst[ex % len(xst)]
                            self.tt("dve", selL[:], iota[:].unsqueeze(1).to_broadcast([128, NL, 256]),
                                    slotT[:, 0:NL, ex:ex + 1].to_broadcast([128, NL, 256]), ALU.is_equal, [r_slotT, rK], [r_selL])
                            if with_ctx:
                                self.tt("dve", selC[:], iota[:, 0:32].unsqueeze(1).to_broadcast([128, 2, 32]),
                                        slotT[:, NL:NT, ex:ex + 1].to_broadcast([128, 2, 32]), ALU.is_equal, [r_slotT, rK], [r_selC])
                            for k in range(8):
                                b = k % 2
                                for ti in range(NL):
                                    self.mm(PS[b][:, 0:256], H[:, ti, k * 128:(k + 1) * 128], selL[:, ti, :], ti == 0, ti == NL - 1,
                                            [rH[ti], r_selL], [rPS[b]])
                                if with_ctx:
                                    for tc in range(2):
                                        self.mm(PS[b][:, 256:288], H[:, NL + tc, k * 128:(k + 1) * 128], selC[:, tc, :], tc == 0, tc == 1,
                                                [rH[NL + tc], r_selC], [rPS[b]])
                                self.cp("act", xs[:, k, 0:S], PS[b][:, 0:S], [rPS[b]], [rxs[k]])

                        def build_sgt(ex):
                            for tb in range(4):
                                b0 = 2 * (tb % 2)
                                ab = abro[tb % 2]
                                rab = r_abro[tb % 2]
                                self.mm(PS[b0][:, :], onehot[:, ex, :], slotb[:, tb * 512:(tb + 1) * 512], True, True, [r_slotb, rK], [rPS[b0]])
                                self.mm(PS[b0 + 1][:, :], onehot[:, ex, :], affTb[:, tb * 512:(tb + 1) * 512], True, True, [r_affTb, rK], [rPS[b0 + 1]])
                                self.cp("act", ab[:], PS[b0 + 1][:, :], [rPS[b0 + 1]], [rab])
                                for half in range(2):
                                    self.stt("dve", sgt[:, half, tb * 512:(tb + 1) * 512], PS[b0][:, :], iotap[:, half:half + 1], ab[:],
                                             ALU.is_equal, ALU.mult, [rPS[b0], rab, rK], [r_sgt[half][tb]])
                            if with_ctx:
                                ab = abro[0]
                                rab = r_abro[0]
                                self.mm(PS[0][:, 0:256], onehot[:, ex, :], slotb[:, SEQ:SEQ + CTX], True, True, [r_slotb, rK], [rPS[0]])
                                self.mm(PS[1][:, 0:256], onehot[:, ex, :], affTb[:, SEQ:SEQ + CTX], True, True, [r_affTb, rK], [rPS[1]])
                                self.cp("act", ab[:, 0:256], PS[1][:, 0:256], [rPS[1]], [rab])
                                self.stt("dve", sgtC[:, :], PS[0][:, 0:256], iotap[:, 0:1], ab[:, 0:256],
                                         ALU.is_equal, ALU.mult, [rPS[0], rab, rK], [r_sgtC])

                        pipelined = cfg.get("pipeline", True)
                        for ex in range(n_exp):
                            if ex == 0 or not pipelined:
                                sel_gather(ex)
                            build_sgt(ex)
                            xs = xst[ex % len(xst)]
                            rxs = r_xst[ex % len(xst)]
                            for fb in range(4):
                                hb = fb % 2
                                sg_, rg_ = ring_next()
                                g3 = sg_[:].rearrange("p (k n) -> p k n", k=8)
                                P.dma("pool", g3, wgv[ex].rearrange("(k p) n -> p k n", p=128)[:, :, fb * 512:(fb + 1) * 512], writes=[rg_])
                                su_, ru_ = ring_next()
                                u3 = su_[:].rearrange("p (k n) -> p k n", k=8)
                                P.dma("pool", u3, wuv[ex].rearrange("(k p) n -> p k n", p=128)[:, :, fb * 512:(fb + 1) * 512], writes=[ru_])
                                sd_, rd_ = ring_next()
                                d3 = sd_[:].rearrange("p (k n) -> p k n", k=4)
                                P.dma("pool", d3, wdv[ex].rearrange("(k p) n -> p k n", p=128)[:, fb * 4:(fb + 1) * 4, :], writes=[rd_])
                                for fc in range(4):
                                    for k in range(8):
                                        self.mm(PS[2][:, 0:S], g3[:, k, fc * 128:(fc + 1) * 128], xs[:, k, 0:S], k == 0, k == 7,
                                                [rg_, rxs[k]], [rPS[2]])
                                    self.act(sg[:, fc % 2, 0:S], PS[2][:, 0:S], AF.Silu, [rPS[2]], [r_sg[fc % 2]])
                                    for k in range(8):
                                        self.mm(PS[3][:, 0:S], u3[:, k, fc * 128:(fc + 1) * 128], xs[:, k, 0:S], k == 0, k == 7,
                                                [ru_, rxs[k]], [rPS[3]])
                                    self.tt("dve", hid[:, hb, fc, 0:S], sg[:, fc % 2, 0:S], PS[3][:, 0:S], ALU.mult,
                                            [r_sg[fc % 2], rPS[3]], [r_hid[hb][fc]])
                                if pipelined and fb == 0 and ex + 1 < n_exp:
                                    sel_gather(ex + 1)
                                for fc in range(4):
                                    f = fb * 4 + fc
                                    for (hh, hn) in halves:
                                        for dh in range(2):
                                            if hh < 2:
                                                o_ = PS[4 + hh * 2 + dh][:, :]
                                                ro_ = rPS[4 + hh * 2 + dh]
                                            else:
                                                o_ = PS[dh][0:32, :]
                                                ro_ = rPS[dh]
                                            self.mm(o_, hid[:, hb, fc, hh * 128:hh * 128 + hn], d3[:, fc, dh * 512:(dh + 1) * 512],
                                                    f == 0, f == 15, [r_hid[hb][fc], rd_], [ro_])
                            for (hh, hn) in halves:
                                for dh in range(2):
                                    if hh < 2:
                                        self.tt("dve", yb[:, hh, dh * 512:(dh + 1) * 512], PS[4 + hh * 2 + dh][:, :], GV[0][:, dh * 512:(dh + 1) * 512],
                                                ALU.mult, [rPS[4 + hh * 2 + dh], rGV[0]], [r_yb[hh][dh]])
                                    else:
                                        self.tt("dve", ybC[0:32, dh * 512:(dh + 1) * 512], PS[dh][0:32, :], GV[1][0:32, dh * 512:(dh + 1) * 512],
                                                ALU.mult, [rPS[dh], rGV[1]], [r_ybC[dh]])
                            n_sc = 0
                            for dh in range(2):
                                for ti in tiles:
                                    b = n_sc % 4
                                    n_sc += 1
                                    if ti < NL:
                                        for hh in range(2):
                                            self.mm(PS[b][:, :], sgt[:, hh, ti * 128:(ti + 1) * 128], yb[:, hh, dh * 512:(dh + 1) * 512],
                                                    hh == 0, hh == 1, [r_sgt[hh][ti // 4], r_yb[hh][dh]], [rPS[b]])
                                    else:
                                        tc = ti - NL
                                        self.mm(PS[b][:, :], sgtC[0:32, tc * 128:(tc + 1) * 128], ybC[0:32, dh * 512:(dh + 1) * 512],
                                                True, True, [r_sgtC, r_ybC[dh]], [rPS[b]])
                                    self.tt("dve", X[:, ti, dh * 512:(dh + 1) * 512], X[:, ti, dh * 512:(dh + 1) * 512], PS[b][:, :], ALU.add,
                                            [rPS[b], rX[ti]], [rX[ti]])
                        P.barrier()

            if cfg.get("do_moe0", True):
                moe(0, True)
            self.tap("xmoe0", X[:], [128, NT, D], rX)

            def mlstm():
                h_sc = 0.125
                with ExitStack() as m0:
                    hTm = self.sb(m0, "hTm", [128, 8, NT * 128], BF16)
                    r_hTm = [P.res(f"hTm{i}") for i in range(NT)]
                    tri = self.sb(m0, "tri", [128, 3, 128], F32)
                    mask2 = self.sb(m0, "mask2", [128, 2, 256], F32)
                    onec = self.sb(m0, "onec", [128, 1], F32)
                    EQ = self.sb(m0, "EQ", [128, NT, 2, 8], F32)
                    EK = self.sb(m0, "EK", [128, NT, 2, 8], F32)
                    EGC = self.sb(m0, "EGC", [128, NT, 2, 4], F32)
                    G1 = self.sb(m0, "G1t", [128, D], F32)
                    NG = self.sb(m0, "NGt", [128, D], F32)
                    bgt = self.sb(m0, "bgt", [128, 32], F32)
                    wgt = self.sb(m0, "wgt", [128, 8, 32], BF16)
                    rT = P.res("mconst", const=True)
                    r_gates, r_SP, r_CS, r_EQ, r_EK, r_EGt, r_EGC = [P.res(n) for n in "gates SP CS EQ EK EGt EGC".split()]
                    P.dma("sp", tri[:], k_tri, writes=[rT])
                    P.dma("sp", mask2[:], k_mask2, writes=[rT])
                    P.op("dve", lambda e: e.memset(onec[:], 1.0), writes=[rT])
                    P.dma("sp", bgt[:], b_gates[0:1, :].partition_broadcast(128), writes=[rT])
                    P.dma("pool", wgt[:], w_in[0].rearrange("(k p) n -> p k n", p=128)[:, :, 3072:3104], writes=[rT])
                    load_vec(G1, rT, modv[1, 2, 0:1, :])
                    P.dma("sp", NG[:], m_norm[0:1, :].partition_broadcast(128), writes=[rT])
                    with ExitStack() as m1:
                        MV = [[self.sb(m1, f"mz{s}{j}", [128, D], F32) for j in range(2)] for s in range(2)]
                        rMV = [[P.res(f"mz{s}{j}") for j in range(2)] for s in range(2)]
                        tmpf = self.sb(m1, "tmph", [128, D], F32)
                        r_tmpf = P.res("tmph")
                        hb = self.sb(m1, "hb", [128, 2, D], BF16)
                        r_hb = [P.res("hb0"), P.res("hb1")]
                        jb = self.sb(m1, "jb", [128, D], BF16)
                        r_jb = P.res("jb")
                        for s in range(2):
                            load_vec(MV[s][0], rMV[s][0], modv[1, 0, s:s + 1, :])
                            load_vec(MV[s][1], rMV[s][1], modv[1, 1, s:s + 1, :])
                        for ti in all_tiles:
                            self.act(jb[:], X[:, ti, :], AF.Square, [rX[ti]], [r_stat, r_jb], accum_out=stat[:, 0, ti:ti + 1])
                        self.act(stat[:, 1, :], stat[:, 0, :], AF.Sqrt, [r_stat, rK], [r_stat], scale=1.0 / D, bias=epsc[:, 0:1])
                        P.op("dve", lambda e: e.reciprocal(out=stat[:, 2, :], in_=stat[:, 1, :]), [r_stat], [r_stat])
                        for ti in all_tiles:
                            s = 0 if ti < NL else 1
                            u = ti % 2
                            self.stt("dve", tmpf[:], X[:, ti, :], stat[:, 2, ti:ti + 1], MV[s][0][:], ALU.mult, ALU.mult,
                                     [rX[ti], r_stat, rMV[s][0]], [r_tmpf])
                            self.tt("dve", hb[:, u, :], tmpf[:], MV[s][1][:], ALU.add, [r_tmpf, rMV[s][1]], [r_hb[u]])
                            psb = PS[u][:, :].bitcast(BF16)
                            for k in range(8):
                                self.tr(psb[:, k * 128:(k + 1) * 128], hb[:, u, k * 128:(k + 1) * 128], identb[:], [r_hb[u], rK], [rPS[u]])
                            self.cp("act", hTm[:, :, ti * 128:(ti + 1) * 128], psb.rearrange("p (k t) -> p k t", k=8), [rPS[u]], [r_hTm[ti]])
                        P.barrier()
                    if cfg.get("ml_stop", 9) <= 1:
                        P.barrier()
                        return
                    m2 = ExitStack()
                    gates = self.sb(m2, "gates", [128, NT, 32], F32)
                    SP = self.sb(m2, "SP", [128, NT, 2, 8], F32)
                    CS = self.sb(m2, "CS", [128, NT, 32], F32)
                    EGt = self.sb(m2, "EGt", [128, NT, 16], F32)
                    for ti in all_tiles:
                        b = 2 + ti // 9
                        off = (ti % 9) * 32
                        for k in range(8):
                            self.mm(PS[b][:, off:off + 32], hTm[:, k, ti * 128:(ti + 1) * 128], wgt[:, k, :], k == 0, k == 7,
                                    [r_hTm[ti], rT], [rPS[b]])
                    for b in range(2):
                        self.tt("dve", gates[:, 9 * b:9 * b + 9, :], PS[2 + b][:, 0:288].rearrange("p (t g) -> p t g", g=32),
                                bgt[:].unsqueeze(1).to_broadcast([128, 9, 32]), ALU.add, [rPS[2 + b], rT], [r_gates])
                    for d in range(2):
                        self.act(SP[:, :, d, :], gates[:, :, 8 + 16 * d:16 + 16 * d], AF.Exp, [r_gates], [r_SP], scale=-1.0)
                    self.act(SP[:], SP[:], AF.Ln, [r_SP, rT], [r_SP], bias=onec[:, 0:1])
                    for ti in all_tiles:
                        b = 4 + ti // 9
                        off = (ti % 9) * 32
                        self.mm(PS[b][:, off:off + 8], tri[:, 0, :], SP[:, ti, 0, :], True, True, [r_SP, rT], [rPS[b]])
                        self.mm(PS[b][:, off + 8:off + 16], tri[:, 1, :], SP[:, ti, 1, :], True, True, [r_SP, rT], [rPS[b]])
                        self.mm(PS[b][:, off + 16:off + 32], tri[:, 2, :], SP[:, ti, :, :].rearrange("p d h -> p (d h)"), True, True,
                                [r_SP, rT], [rPS[b]])
                    for b in range(2):
                        self.cp("dve", CS[:, 9 * b:9 * b + 9, :], PS[4 + b][:, 0:288].rearrange("p (t g) -> p t g", g=32), [rPS[4 + b]], [r_CS])
                    self.act(EQ[:].rearrange("p t d h -> p t (d h)"), CS[:, :, 0:16], AF.Exp, [r_CS], [r_EQ], scale=-1.0)
                    for d in range(2):
                        self.tt("dve", EK[:, :, d, :], gates[:, :, 16 * d:16 * d + 8], CS[:, :, 8 * d:8 * d + 8], ALU.add, [r_gates, r_CS], [r_EK])
                    self.act(EK[:], EK[:], AF.Exp, [r_EK], [r_EK])
                    self.ts("dve", EK[:], EK[:], h_sc, None, ALU.mult, None, [r_EK], [r_EK])
                    self.act(EGt[:], CS[:, :, 16:32], AF.Exp, [r_CS], [r_EGt], scale=-1.0)
                    eg5 = EGt[:].rearrange("p t (d g l) -> p t d g l", d=2, g=4, l=2)
                    self.cp("dve", EGC[0:64], eg5[0:64, :, :, :, 0], [r_EGt], [r_EGC])
                    self.cp("dve", EGC[64:128], eg5[64:128, :, :, :, 1], [r_EGt], [r_EGC])
                    self.tap("mgates", gates[:], [128, NT, 32], [r_gates])
                    self.tap("mEQ", EQ[:], [128, NT, 2, 8], [r_EQ])
                    self.tap("mEK", EK[:], [128, NT, 2, 8], [r_EK])
                    self.tap("mEGC", EGC[:], [128, NT, 2, 4], [r_EGC])
                    P.barrier()
                    m2.close()
                    if cfg.get("ml_stop", 9) <= 2:
                        return
                    with ExitStack() as m3:
                        Qs = self.sb(m3, "Qs", [128, NT, 128], BF16)
                        Ks = self.sb(m3, "Ks", [128, NT, 128], BF16)
                        Vh = self.sb(m3, "Vh", [128, NT, 2, 144], BF16)
                        HF = self.sb(m3, "HF", [128, NL, 256], F32)
                        r_Q = [P.res(f"Q{i}") for i in range(NT)]
                        r_V = [P.res(f"V{i}") for i in range(NT)]
                        r_HF = [P.res(f"HF{i}") for i in range(NL)]
                        qs = [self.sb(m3, f"qs{d}", [128, 128], BF16) for d in range(2)]
                        ks = [self.sb(m3, f"ks{d}", [128, 128], BF16) for d in range(2)]
                        qkT = [self.sb(m3, f"qkT{d}", [128, 3, 128], BF16) for d in range(2)]
                        ST = [self.sb(m3, f"ST{d}", [128, 2, 128], BF16) for d in range(2)]
                        Cf = [self.sb(m3, f"Cf{d}", [128, 144], F32) for d in range(2)]
                        Cb = [self.sb(m3, f"Cb{d}", [128, 144], BF16) for d in range(2)]
                        ctmp = [self.sb(m3, f"ctmp{d}", [128, 144], F32) for d in range(2)]
                        rr = [self.sb(m3, f"rr{d}", [128, 4], F32) for d in range(2)]
                        r_qs, r_ks, r_qkT, r_ST, r_Cf, r_Cb, r_ctmp, r_rr = [[P.res(f"{n}{d}") for d in range(2)]
                                                                             for n in "qs ks qkT ST Cf Cb ctmp rr".split()]
                        og = self.sb(m3, "og", [128, 2, 256], F32)
                        sq = [self.sb(m3, f"sq{i}", [128, 256], F32) for i in range(2)]
                        ss = [self.sb(m3, f"ss{i}", [128, 8], F32) for i in range(2)]
                        t1 = [self.sb(m3, "t1s", [128, 256], F32)] * 2
                        ho = [self.sb(m3, f"ho{i}", [128, 256], BF16) for i in range(2)]
                        hoT = [self.sb(m3, f"hoT{i}", [128, 2, 128], BF16) for i in range(2)]
                        y5 = [self.sb(m3, "y5s", [128, 1, 512], F32)] * 2
                        r_og = [P.res("og0"), P.res("og1")]
                        r_sq, r_ss, r_ho, r_hoT = [[P.res(f"{n}{i}") for i in range(2)] for n in "sq ss ho hoT".split()]
                        r_t1 = [P.res("t1s")] * 2
                        r_y5 = [P.res("y5s")] * 2
                        for ti in all_tiles:
                            P.op("dve", lambda e, ti=ti: e.memset(Vh[:, ti, :, :].rearrange("p h v -> p (h v)"), 0.0), writes=[r_V[ti]])
                            for hl in range(2):
                                P.op("dve", lambda e, ti=ti, hl=hl: e.memset(Vh[:, ti, hl, 128:129], 1.0), writes=[r_V[ti]])
                        w3 = w_in[0].rearrange("(k p) n -> p k n", p=128)
                        order = [[16, 17] + list(range(16)), [17, 16] + list(range(15, -1, -1))]
                        for hg in range(cfg.get("n_hg", 4)):
                            slot, rs = ring_next()
                            wqkv = slot[:].rearrange("p (k n) -> p k n", k=8)
                            P.dma("pool", wqkv[:, :, 0:128], w3[:, :, hg * 128:(hg + 1) * 128], writes=[rs])
                            P.dma("pool", wqkv[:, :, 128:256], w3[:, :, 512 + hg * 128:512 + (hg + 1) * 128], writes=[rs])
                            P.dma("pool", wqkv[:, :, 256:512], w3[:, :, 1024 + hg * 256:1024 + (hg + 1) * 256], writes=[rs])
                            pj = cfg.get("pj_upto", 9)
                            for ti in all_tiles:
                                b = 4 + ti % 2
                                if pj < 2:
                                    break
                                for k in range(8):
                                    self.mm(PS[b][:, :], hTm[:, k, ti * 128:(ti + 1) * 128], wqkv[:, k, :], k == 0, k == 7,
                                            [r_hTm[ti], rs], [rPS[b]])
                                if pj < 3:
                                    continue
                                self.cp("act", Qs[:, ti, :], PS[b][:, 0:128], [rPS[b]], [r_Q[ti]])
                                self.cp("act", Ks[:, ti, :], PS[b][:, 128:256], [rPS[b]], [r_Q[ti]])
                                if pj < 4:
                                    continue
                                for hl in range(2):
                                    self.cp("act", Vh[:, ti, hl, 0:128], PS[b][:, 256 + 128 * hl:384 + 128 * hl], [rPS[b]], [r_V[ti]])
                            for ti in range(NL):
                                P.op("dve", lambda e, ti=ti: e.memset(HF[:, ti, :], 0.0), writes=[r_HF[ti]])
                            for d in range(2):
                                if hg == 0:
                                    P.op("dve", lambda e, d=d: e.memset(qkT[d][:].rearrange("p a t -> p (a t)"), 0.0), writes=[r_qkT[d]])
                                P.op("dve", lambda e, d=d: e.memset(Cf[d][:], 0.0), writes=[r_Cf[d]])
                                P.op("dve", lambda e, d=d: e.memset(Cb[d][:], 0.0), writes=[r_Cb[d]])
                            h0 = 2 * hg
                            if cfg.get("ml_stop", 9) <= 3:
                                P.barrier()
                                return
                            for step in range(cfg.get("ml_steps", NT)):
                                for d in range(2):
                                    c = order[d][step]
                                    lat_c = c < NL
                                    self.tt("dve", ks[d][:].rearrange("p (h k) -> p h k", h=2), Ks[:, c, :].rearrange("p (h k) -> p h k", h=2),
                                            EK[:, c, d, h0:h0 + 2].unsqueeze(2).to_broadcast([128, 2, 64]), ALU.mult,
                                            [r_Q[c], r_EK], [r_ks[d]])
                                    if lat_c:
                                        self.tt("dve", qs[d][:].rearrange("p (h k) -> p h k", h=2), Qs[:, c, :].rearrange("p (h k) -> p h k", h=2),
                                                EQ[:, c, d, h0:h0 + 2].unsqueeze(2).to_broadcast([128, 2, 64]), ALU.mult,
                                                [r_Q[c], r_EQ], [r_qs[d]])
                                        psb = PS[d][:, :].bitcast(BF16)
                                        self.tr(psb[:, 0:128], qs[d][:], identb[:], [r_qs[d], rK], [rPS[d]])
                                        self.tr(psb[:, 128:256], ks[d][:], identb[:], [r_ks[d], rK], [rPS[d]])
                                        self.cp("act", qkT[d][0:64, 0, :], psb[0:64, 0:128], [rPS[d]], [r_qkT[d]])
                                        self.cp("act", qkT[d][64:128, 1, :], psb[64:128, 0:128], [rPS[d]], [r_qkT[d]])
                                        self.cp("act", qkT[d][:, 2, :], psb[:, 128:256], [rPS[d]], [r_qkT[d]])
                                        for hl in range(2):
                                            self.mm(PS[2 + d][:, hl * 128:(hl + 1) * 128], qkT[d][:, 2, :],
                                                    qkT[d][:, hl, :], True, True, [r_qkT[d]], [rPS[2 + d]])
                                        self.tt("dve", ST[d][:].rearrange("p h t -> p (h t)"), PS[2 + d][:, 0:256], mask2[:, d, :], ALU.mult,
                                                [rPS[2 + d], rT], [r_ST[d]])
                                        for hl in range(2):
                                            self.mm(PS[4 + d][:, hl * 144:hl * 144 + 129], qkT[d][:, hl, :],
                                                    Cb[d][:, 0:129], True, False, [r_qkT[d], r_Cb[d]], [rPS[4 + d]])
                                            self.mm(PS[4 + d][:, hl * 144:hl * 144 + 129], ST[d][:, hl, :], Vh[:, c, hl, 0:129], False, True,
                                                    [r_ST[d], r_V[c]], [rPS[4 + d]])
                                        for hl in range(2):
                                            self.act(rr[d][:, hl:hl + 1], PS[4 + d][:, hl * 144 + 128:hl * 144 + 129], AF.Abs, [rPS[4 + d]], [r_rr[d]])
                                        self.ts("dve", rr[d][:, 0:2], rr[d][:, 0:2], 1.0, None, ALU.max, None, [r_rr[d]], [r_rr[d]])
                                        P.op("dve", lambda e, d=d: e.reciprocal(out=rr[d][:, 2:4], in_=rr[d][:, 0:2]), [r_rr[d]], [r_rr[d]])
                                        for hl in range(2):
                                            self.stt("dve", HF[:, c, hl * 128:(hl + 1) * 128], PS[4 + d][:, hl * 144:hl * 144 + 128],
                                                     rr[d][:, 2 + hl:3 + hl], HF[:, c, hl * 128:(hl + 1) * 128], ALU.mult, ALU.add,
                                                     [rPS[4 + d], r_rr[d], r_HF[c]], [r_HF[c]])
                                    self.mm(PS[6 + d][:, 0:288], ks[d][:], Vh[:, c, :, :].rearrange("p h v -> p (h v)"), True, True,
                                            [r_ks[d], r_V[c]], [rPS[6 + d]])
                                    self.tt("dve", ctmp[d][0:64, :], Cf[d][0:64, :], PS[6 + d][0:64, 0:144], ALU.add, [r_Cf[d], rPS[6 + d]], [r_ctmp[d]])
                                    self.tt("dve", ctmp[d][64:128, :], Cf[d][64:128, :], PS[6 + d][64:128, 144:288], ALU.add,
                                            [r_Cf[d], rPS[6 + d]], [r_ctmp[d]])
                                    self.ts("dve", Cf[d][:], ctmp[d][:], EGC[:, c, d, hg:hg + 1], None, ALU.mult, None, [r_ctmp[d], r_EGC], [r_Cf[d]])
                                    self.cp("act", Cb[d][:], Cf[d][:], [r_Cf[d]], [r_Cb[d]])
                            if hg == 0:
                                self.tap("mHF", HF[:], [128, NL, 256], r_HF)
                            if cfg.get("ml_stop", 9) <= 4:
                                P.barrier()
                                return
                            slot2, rs2 = ring_next()
                            wo = slot2[:, 0:2048].rearrange("p (k n) -> p k n", k=8)
                            wout = slot2[:, 2048:4096].rearrange("p (k n) -> p k n", k=2)
                            P.dma("pool", wo, w3[:, :, 2048 + hg * 256:2048 + (hg + 1) * 256], writes=[rs2])
                            P.dma("pool", wout, w_out[0].rearrange("(k p) n -> p k n", p=128)[:, 2 * hg:2 * hg + 2, :], writes=[rs2])
                            for ti in range(NL):
                                u = ti % 2
                                bo = 4 * u
                                for k in range(8):
                                    self.mm(PS[bo][:, 0:256], hTm[:, k, ti * 128:(ti + 1) * 128], wo[:, k, :], k == 0, k == 7,
                                            [r_hTm[ti], rs2], [rPS[bo]])
                                self.act(og[:, u, :], PS[bo][:, 0:256], AF.Sigmoid, [rPS[bo]], [r_og[u]])
                                self.tt("dve", sq[u][:], HF[:, ti, :], HF[:, ti, :], ALU.mult, [r_HF[ti]], [r_sq[u]])
                                P.op("dve", lambda e, u=u: e.tensor_reduce(out=ss[u][:, 0:2], in_=sq[u][:].rearrange("p (h v) -> p h v", h=2),
                                                                          axis=AX.X, op=ALU.add), [r_sq[u]], [r_ss[u]])
                                self.act(ss[u][:, 2:4], ss[u][:, 0:2], AF.Sqrt, [r_ss[u], rK], [r_ss[u]], scale=1.0 / 128, bias=epsc[:, 0:1])
                                P.op("dve", lambda e, u=u: e.reciprocal(out=ss[u][:, 4:6], in_=ss[u][:, 2:4]), [r_ss[u]], [r_ss[u]])
                                self.tt("dve", t1[u][:].rearrange("p (h v) -> p h v", h=2), HF[:, ti, :].rearrange("p (h v) -> p h v", h=2),
                                        ss[u][:, 4:6].unsqueeze(2).to_broadcast([128, 2, 128]), ALU.mult, [r_HF[ti], r_ss[u]], [r_t1[u]])
                                self.tt("dve", t1[u][:], t1[u][:], NG[:, hg * 256:(hg + 1) * 256], ALU.mult, [r_t1[u], rT], [r_t1[u]])
                                self.tt("dve", ho[u][:], t1[u][:], og[:, u, :], ALU.mult, [r_t1[u], r_og[u]], [r_ho[u]])
                                psb = PS[bo + 1][:, :].bitcast(BF16)
                                for c2 in range(2):
                                    self.tr(psb[:, c2 * 128:(c2 + 1) * 128], ho[u][:, c2 * 128:(c2 + 1) * 128], identb[:], [r_ho[u], rK], [rPS[bo + 1]])
                                self.cp("act", hoT[u][:].rearrange("p a t -> p (a t)"), psb[:, 0:256], [rPS[bo + 1]], [r_hoT[u]])
                                for dh in range(2):
                                    for c2 in range(2):
                                        self.mm(PS[bo + 2 + dh][:, :], hoT[u][:, c2, :], wout[:, c2, dh * 512:(dh + 1) * 512], c2 == 0, c2 == 1,
                                                [r_hoT[u], rs2], [rPS[bo + 2 + dh]])
                                    self.tt("dve", y5[u][:, 0, :], PS[bo + 2 + dh][:, :], G1[:, dh * 512:(dh + 1) * 512], ALU.mult,
                                            [rPS[bo + 2 + dh], rT], [r_y5[u]])
                                    self.tt("dve", X[:, ti, dh * 512:(dh + 1) * 512], X[:, ti, dh * 512:(dh + 1) * 512], y5[u][:, 0, :], ALU.add,
                                            [r_y5[u], rX[ti]], [rX[ti]])
                        P.barrier()

            if cfg.get("do_mlstm", True):
                mlstm()
            self.tap("xmix1", X[:], [128, NT, D], rX)
            if cfg.get("do_moe1", True):
                moe(1, False)

            with ExitStack() as s5:
                FV = self.sb(s5, "fv", [128, D], F32)
                r_FV = P.res("fv")
                fj = self.sb(s5, "fj", [128, D], BF16)
                r_fj = P.res("fj")
                P.dma("sp", FV[:], final_norm[0:1, :].partition_broadcast(128), writes=[r_FV])
                lat = list(range(NL))
                for ti in lat:
                    self.act(fj[:], X[:, ti, :], AF.Square, [rX[ti]], [r_stat, r_fj], accum_out=stat[:, 0, ti:ti + 1])
                self.act(stat[:, 1, :], stat[:, 0, :], AF.Sqrt, [r_stat, rK], [r_stat], scale=1.0 / D, bias=epsc[:, 0:1])
                P.op("dve", lambda e: e.reciprocal(out=stat[:, 2, :], in_=stat[:, 1, :]), [r_stat], [r_stat])
                ov = out_d.rearrange("(t p) d -> p t d", p=128)
                for ti in lat:
                    self.stt("dve", X[:, ti, :], X[:, ti, :], stat[:, 2, ti:ti + 1], FV[:], ALU.mult, ALU.mult,
                             [rX[ti], r_stat, r_FV], [rX[ti]])
                    P.dma("sp", ov[:, ti, :], X[:, ti, :], reads=[rX[ti]])
                P.emit(st)
        return nc


_CACHE = {}


def _layout_inputs(inputs, b):
    m = {}
    m["x"] = np.ascontiguousarray(inputs["x"][b])
    m["ctx"] = np.ascontiguousarray(inputs["ctx"][b])
    cc = np.stack([inputs["c"][b], inputs["c_ctx"]], axis=-1)
    m["cc"] = np.ascontiguousarray(cc.reshape(8, 128, 2).transpose(1, 0, 2))
    for k in ("ada_w", "ada_b", "norm_mix", "norm_ffn", "pool_w", "pool_scale", "mlstm_w_in", "mlstm_b_gates",
              "mlstm_norm", "mlstm_w_out", "moe_router", "moe_w_gate", "moe_w_up", "moe_w_down"):
        m[k] = inputs[k]
    m["final_norm"] = inputs["final_norm"].reshape(1, D)
    m.update(host_constants())
    return m


def kernel(**inputs):
    inputs = {k: np.asarray(v) for k, v in inputs.items()}
    n = 8
    bld = Builder()
    nc = bld.build()
    in_maps = [_layout_inputs(inputs, b) for b in range(n)]
    res = run_bass_kernel_spmd(nc, in_maps, core_ids=list(range(n)))
    return np.stack([r["out"] for r in res.results], axis=0).astype(np.float32)
```

```python
import numpy as np
import ml_dtypes
from contextlib import ExitStack
import concourse.bass as bass
import concourse.mybir as mybir
from concourse.bass_utils import run_bass_kernel_spmd

F32 = mybir.dt.float32
BF16 = mybir.dt.bfloat16
ALU = mybir.AluOpType
AF = mybir.ActivationFunctionType
AX = mybir.AxisListType

D = 1024
SEQ = 2048
CTX = 256
NT = 18
NL = 16
E = 16
FH = 2048
POOL_WINDOWS = (2, 4, 8, 16)
EPS = 1e-6

SAME_ENGINE_SYNC = True
ENGS = ("pe", "dve", "act", "pool", "sp")


class Res:
    __slots__ = ("name", "w", "rs", "const")

    def __init__(self, name, const=False):
        self.name = name
        self.w = None
        self.rs = []
        self.const = const


class Op:
    __slots__ = ("eng", "fn", "deps", "signal", "is_dma", "lane", "cum", "sig_idx", "lane_prev")

    def __init__(self, eng, fn, is_dma=False):
        self.eng = eng
        self.fn = fn
        self.deps = set()
        self.signal = False
        self.is_dma = is_dma
        self.lane = None
        self.cum = 0
        self.sig_idx = 0
        self.lane_prev = 0


class Prog:
    def __init__(self, nc, n_lanes=8):
        self.nc = nc
        self.ops = {e: [] for e in ENGS}
        self.last = {e: None for e in ENGS}
        self.n_lanes = n_lanes
        self.lane_rr = {"sp": 0, "pool": 0, "act": 0}
        self.lane_cum = {}
        self.lane_last = {}
        self.pending = {e: set() for e in ENGS}
        self.nres = 0

    def res(self, name=None, const=False):
        self.nres += 1
        return Res(name or f"r{self.nres}", const)

    def _track(self, op, reads, writes):
        for r in reads:
            if r.w is not None:
                op.deps.add(r.w)
            if not r.const:
                r.rs.append(op)
        for r in writes:
            if r.w is not None:
                op.deps.add(r.w)
            for o in r.rs:
                op.deps.add(o)
            r.w = op
            r.rs = []
        if self.pending[op.eng]:
            op.deps |= self.pending[op.eng]
            self.pending[op.eng] = set()
        op.deps.discard(op)

    def op(self, eng, fn, reads=(), writes=()):
        o = Op(eng, fn)
        self._track(o, reads, writes)
        self.ops[eng].append(o)
        self.last[eng] = o
        return o

    def dma(self, q, out, in_, reads=(), writes=(), **kw):
        o = Op(q, None, is_dma=True)

        def fn(e, out=out, in_=in_, kw=kw):
            return e.dma_start(out=out, in_=in_, **kw)
        o.fn = fn
        lane = (q, self.lane_rr[q] % self.n_lanes)
        self.lane_rr[q] += 1
        o.lane = lane
        o.lane_prev = self.lane_cum.get(lane, 0)
        o.cum = o.lane_prev + 1
        self.lane_cum[lane] = o.cum
        self.lane_last[lane] = o
        self._track(o, reads, writes)
        self.ops[q].append(o)
        self.last[q] = o
        return o

    def barrier(self):
        deps = set(o for o in self.last.values() if o is not None)
        deps |= set(self.lane_last.values())
        for e in ENGS:
            self.pending[e] |= deps

    def emit(self, stack):
        nc = self.nc
        for e in ENGS:
            for o in self.ops[e]:
                for d in o.deps:
                    if d.is_dma:
                        continue
                    if d.eng != o.eng or (SAME_ENGINE_SYNC and o.eng != "pe"):
                        d.signal = True
        sems = {}
        for e in ENGS:
            sems[e] = stack.enter_context(nc.semaphore(f"s_{e}"))
            k = 0
            for o in self.ops[e]:
                if o.signal and not o.is_dma:
                    k += 1
                    o.sig_idx = k
        lane_sems = {}
        for lane in self.lane_cum:
            lane_sems[lane] = stack.enter_context(nc.semaphore(f"l_{lane[0]}{lane[1]}"))

        def run(ename, eng):
            waited = {}
            for o in self.ops[ename]:
                need = {}
                for d in o.deps:
                    if d.is_dma:
                        key = ("L", d.lane)
                        val = 16 * d.cum
                    else:
                        if d.eng == ename and (ename == "pe" or not SAME_ENGINE_SYNC):
                            continue
                        key = ("E", d.eng)
                        val = d.sig_idx
                    if val > need.get(key, 0):
                        need[key] = val
                if o.is_dma and o.lane_prev > 0:
                    key = ("L", o.lane)
                    val = 16 * o.lane_prev
                    if val > need.get(key, 0):
                        need[key] = val
                for key, val in need.items():
                    if waited.get(key, 0) >= val:
                        continue
                    waited[key] = val
                    s = lane_sems[key[1]] if key[0] == "L" else sems[key[1]]
                    eng.wait_ge(s, val)
                ins = o.fn(eng)
                if o.is_dma:
                    ins.then_inc(lane_sems[o.lane], 16)
                elif o.signal:
                    ins.then_inc(sems[ename], 1)
            for lane, cum in self.lane_cum.items():
                if lane[0] == ename and waited.get(("L", lane), 0) < 16 * cum:
                    eng.wait_ge(lane_sems[lane], 16 * cum)

        with nc.Block() as block:
            @block.sync
            def _(e):
                run("sp", e)

            @block.tensor
            def _(e):
                run("pe", e)

            @block.vector
            def _(e):
                run("dve", e)

            @block.scalar
            def _(e):
                run("act", e)

            @block.gpsimd
            def _(e):
                run("pool", e)


def _pool_constants():
    blocks = {}
    blk_list = []
    lat_map = {}
    ctx_map = {}
    invcnt = np.zeros((128, NT, 4), np.float32)

    def add(mat):
        key = mat.tobytes()
        if key not in blocks:
            blocks[key] = len(blk_list)
            blk_list.append(mat)
        return blocks[key]

    r = np.arange(32)
    wv = np.arange(64)
    for j, w in enumerate(POOL_WINDOWS):
        h = w // 2
        Br = ((r[None, :] >= r[:, None] - h) & (r[None, :] < r[:, None] + h)).astype(np.float32)
        Bw = ((wv[None, :] >= wv[:, None] - h) & (wv[None, :] < wv[:, None] + h)).astype(np.float32)
        M = np.kron(Br, Bw)
        cnt = M.sum(1)
        M = M - np.diag(cnt)
        invcnt[:, :NL, j] = (1.0 / cnt).reshape(NL, 128).T
        for io in range(NL):
            for ii in range(NL):
                b = M[io * 128:(io + 1) * 128, ii * 128:(ii + 1) * 128]
                if np.any(b):
                    lat_map[(j, io, ii)] = add(np.ascontiguousarray(b.T))
        t = np.arange(CTX)
        Mc = ((t[None, :] >= t[:, None] - h) & (t[None, :] < t[:, None] + h)).astype(np.float32)
        cc = Mc.sum(1)
        Mc = Mc - np.diag(cc)
        invcnt[:, NL:, j] = (1.0 / cc).reshape(2, 128).T
        for io in range(2):
            for ii in range(2):
                b = Mc[io * 128:(io + 1) * 128, ii * 128:(ii + 1) * 128]
                if np.any(b):
                    ctx_map[(j, io, ii)] = add(np.ascontiguousarray(b.T))
    blks = np.stack(blk_list).astype(ml_dtypes.bfloat16)
    assert np.all(blks.astype(np.float32) == np.stack(blk_list))
    return blks, lat_map, ctx_map, invcnt


_PC = None


def pool_constants():
    global _PC
    if _PC is None:
        _PC = _pool_constants()
    return _PC


def host_constants():
    blks, lat_map, ctx_map, invcnt = pool_constants()
    c = {}
    c["k_pblk"] = np.ascontiguousarray(blks.transpose(1, 0, 2))
    c["k_invcnt"] = invcnt
    c["k_identb"] = np.eye(128, dtype=np.float32).astype(ml_dtypes.bfloat16)
    c["k_identf"] = np.eye(128, dtype=np.float32)
    c["k_iota"] = np.tile(np.arange(256, dtype=np.float32)[None, :], (128, 1))
    ip = np.zeros((128, 4), np.float32)
    ip[:, 0] = np.arange(128)
    ip[:, 1] = np.arange(128) + 128
    c["k_iotap"] = ip
    oh = np.zeros((16, 16, 128), np.float32)
    for e in range(16):
        oh[e, e, :] = 1.0
    c["k_onehot"] = oh.astype(ml_dtypes.bfloat16)
    c["k_ones"] = np.ones((128, 2048), np.float32)
    tri = np.zeros((128, 3, 128), np.float32)
    tri[:, 0, :] = np.triu(np.ones((128, 128), np.float32))
    tri[:, 1, :] = np.tril(np.ones((128, 128), np.float32))
    tri[:, 2, :] = 1.0
    c["k_tri"] = tri
    c["k_mask2"] = np.ascontiguousarray(np.concatenate([tri[:, 0:2, :], tri[:, 0:2, :]], axis=2))
    return c


class Builder:
    def __init__(self, cfg=None):
        self.cfg = cfg or {}
        self.nc = bass.Bass("TRN2", target_bir_lowering=False)
        self.P = Prog(self.nc)
        self.taps = {}

    def sb(self, st, name, shape, dt):
        self._nsb = getattr(self, "_nsb", 0) + 1
        return st.enter_context(self.nc.sbuf_tensor(f"{name}_{self._nsb}", list(shape), dt))

    def din(self, name, shape, dt=F32):
        return self.nc.dram_tensor(name, list(shape), dt, kind="ExternalInput").ap()

    def mm(self, out, lhsT, rhs, start, stop, reads, writes):
        self.P.op("pe", lambda e: e.matmul(out, lhsT=lhsT, rhs=rhs, start=start, stop=stop), reads, writes)

    def tr(self, out, in_, ident, reads, writes):
        self.P.op("pe", lambda e: e.transpose(out, in_, ident), reads, writes)

    def act(self, out, in_, func, reads, writes, **kw):
        self.P.op("act", lambda e: e.activation(out=out, in_=in_, func=func, **kw), reads, writes)

    def tt(self, eng, out, in0, in1, op, reads, writes):
        self.P.op(eng, lambda e: e.tensor_tensor(out=out, in0=in0, in1=in1, op=op), reads, writes)

    def ts(self, eng, out, in0, s1, s2, op0, op1, reads, writes):
        if s2 is None:
            self.P.op(eng, lambda e: e.tensor_scalar(out=out, in0=in0, scalar1=s1, scalar2=None, op0=op0), reads, writes)
        else:
            self.P.op(eng, lambda e: e.tensor_scalar(out=out, in0=in0, scalar1=s1, scalar2=s2, op0=op0, op1=op1), reads, writes)

    def stt(self, eng, out, in0, scalar, in1, op0, op1, reads, writes):
        self.P.op(eng, lambda e: e.scalar_tensor_tensor(out=out, in0=in0, scalar=scalar, in1=in1, op0=op0, op1=op1), reads, writes)

    def cp(self, eng, out, in_, reads, writes):
        if eng == "act":
            self.P.op("act", lambda e: e.copy(out=out, in_=in_), reads, writes)
        else:
            self.P.op(eng, lambda e: e.tensor_copy(out=out, in_=in_), reads, writes)

    def tap(self, name, sb_ap, shape, reads, dt=F32):
        if name not in self.cfg.get("taps", ()):
            return
        d = self.nc.dram_tensor("tap_" + name, list(shape), dt, kind="ExternalOutput").ap()
        self.P.dma("sp", d, sb_ap, reads=reads)
        self.taps[name] = "tap_" + name

    def build(self):
        nc, P, cfg = self.nc, self.P, self.cfg
        blks, lat_map, ctx_map, _ = pool_constants()
        NB = blks.shape[0]
        x_d = self.din("x", [SEQ, D])
        ctx_d = self.din("ctx", [CTX, D])
        cc_d = self.din("cc", [128, 8, 2])
        ada_w = self.din("ada_w", [2, D, 6 * D])
        ada_b = self.din("ada_b", [2, 6 * D])
        norm_mix = self.din("norm_mix", [2, D])
        norm_ffn = self.din("norm_ffn", [2, D])
        pool_w = self.din("pool_w", [1, 4, 256, 256])
        pool_scale = self.din("pool_scale", [1, D])
        w_in = self.din("mlstm_w_in", [1, D, 3104])
        b_gates = self.din("mlstm_b_gates", [1, 32])
        m_norm = self.din("mlstm_norm", [1, D])
        w_out = self.din("mlstm_w_out", [1, D, D])
        router = self.din("moe_router", [2, D, E])
        EW = cfg.get("ew", E)
        w_gate = self.din("moe_w_gate", [2, EW, D, FH])
        w_up = self.din("moe_w_up", [2, EW, D, FH])
        w_down = self.din("moe_w_down", [2, EW, FH, D])
        final_norm = self.din("final_norm", [1, D])
        k_pblk = self.din("k_pblk", [128, NB, 128], BF16)
        k_invcnt = self.din("k_invcnt", [128, NT, 4])
        k_identb = self.din("k_identb", [128, 128], BF16)
        k_identf = self.din("k_identf", [128, 128])
        k_iota = self.din("k_iota", [128, 256])
        k_iotap = self.din("k_iotap", [128, 4])
        k_onehot = self.din("k_onehot", [16, 16, 128], BF16)
        k_ones = self.din("k_ones", [128, 2048])
        k_tri = self.din("k_tri", [128, 3, 128])
        k_mask2 = self.din("k_mask2", [128, 2, 256])
        out_d = nc.dram_tensor("out", [SEQ, D], F32, kind="ExternalOutput").ap()
        modv = nc.dram_tensor("modv", [2, 6, 2, D], F32).ap()

        with ExitStack() as st:
            X = self.sb(st, "X", [128, NT, D], F32)
            rX = [P.res(f"X{i}") for i in range(NT)]
            identb = self.sb(st, "identb", [128, 128], BF16)
            identf = self.sb(st, "identf", [128, 128], F32)
            iota = self.sb(st, "iota", [128, 256], F32)
            iotap = self.sb(st, "iotap", [128, 4], F32)
            epsc = self.sb(st, "epsc", [128, 1], F32)
            stat = self.sb(st, "stat", [128, 4, NT], F32)
            rK = P.res("consts", const=True)
            r_stat = P.res("stat")
            for t_, d_ in ((identb, k_identb), (identf, k_identf), (iota, k_iota), (iotap, k_iotap)):
                P.dma("sp", t_[:], d_, writes=[rK])
            P.op("dve", lambda e: e.memset(epsc[:], EPS), writes=[rK])
            PS = [st.enter_context(nc.psum_tensor(f"ps{i}", [128, 512], F32)) for i in range(8)]
            rPS = [P.res(f"ps{i}") for i in range(8)]
            NSLOT = cfg.get("nslot", 4)
            RING = [self.sb(st, f"ring{i}", [128, 4096], BF16) for i in range(NSLOT)]
            rRING = [P.res(f"ring{i}") for i in range(NSLOT)]
            self.ring_i = 0

            def ring_next():
                i = self.ring_i % NSLOT
                self.ring_i += 1
                return RING[i], rRING[i]

            xv = x_d.rearrange("(t p) d -> p t d", p=128)
            cv = ctx_d.rearrange("(t p) d -> p t d", p=128)
            for ti in range(NL):
                P.dma("sp", X[:, ti, :], xv[:, ti, :], writes=[rX[ti]])
            for ti in range(2):
                P.dma("sp", X[:, NL + ti, :], cv[:, ti, :], writes=[rX[NL + ti]])

            with ExitStack() as s0:
                cc_t = self.sb(s0, "cc_t", [128, 8, 2], F32)
                scb = self.sb(s0, "scb", [128, 8, 2], BF16)
                modrow = self.sb(s0, "modrow", [2, 6 * D], F32)
                adab = self.sb(s0, "adab", [2, 6 * D], F32)
                vec = self.sb(s0, "vec", [2, 6, D], F32)
                grow = self.sb(s0, "grow", [2, 3, D], F32)
                r_cc, r_scb, r_mod, r_adab, r_vec, r_grow = [P.res(n) for n in "cc scb mod adab vec grow".split()]
                P.dma("sp", cc_t[:], cc_d, writes=[r_cc])
                self.act(scb[:], cc_t[:], AF.Silu, [r_cc], [r_scb])
                r_modv = P.res("modv")
                self.r_modv = r_modv
                for i in range(2):
                    for s in range(2):
                        P.dma("sp", adab[s:s + 1, :], ada_b[i:i + 1, :], writes=[r_adab])
                        P.dma("sp", grow[s:s + 1, 0, :], norm_mix[i:i + 1, :], writes=[r_grow])
                        P.dma("sp", grow[s:s + 1, 1, :], norm_ffn[i:i + 1, :], writes=[r_grow])
                        if i == 0:
                            P.dma("sp", grow[s:s + 1, 2, :], pool_scale[0:1, :], writes=[r_grow])
                    awv = ada_w[i].rearrange("(k p) n -> p k n", p=128)
                    for nb in range(12):
                        slot, rs = ring_next()
                        sl3 = slot[:].rearrange("p (k n) -> p k n", k=8)
                        P.dma("pool", sl3, awv[:, :, nb * 512:(nb + 1) * 512], writes=[rs])
                        pb = nb % 2
                        for k in range(8):
                            self.mm(PS[pb][0:2, :], scb[:, k, :], sl3[:, k, :], k == 0, k == 7, [r_scb, rs], [rPS[pb]])
                        self.tt("dve", modrow[:, nb * 512:(nb + 1) * 512], PS[pb][0:2, :], adab[:, nb * 512:(nb + 1) * 512],
                                ALU.add, [rPS[pb], r_adab], [r_mod])
                    self.stt("dve", vec[:, 0, :], modrow[:, D:2 * D], 1.0, grow[:, 0, :], ALU.add, ALU.mult, [r_mod, r_grow], [r_vec])
                    self.cp("dve", vec[:, 1, :], modrow[:, 0:D], [r_mod], [r_vec])
                    if i == 0:
                        self.tt("dve", vec[:, 2, :], modrow[:, 2 * D:3 * D], grow[:, 2, :], ALU.mult, [r_mod, r_grow], [r_vec])
                    else:
                        self.cp("dve", vec[:, 2, :], modrow[:, 2 * D:3 * D], [r_mod], [r_vec])
                    self.stt("dve", vec[:, 3, :], modrow[:, 4 * D:5 * D], 1.0, grow[:, 1, :], ALU.add, ALU.mult, [r_mod, r_grow], [r_vec])
                    self.cp("dve", vec[:, 4, :], modrow[:, 3 * D:4 * D], [r_mod], [r_vec])
                    self.cp("dve", vec[:, 5, :], modrow[:, 5 * D:6 * D], [r_mod], [r_vec])
                    P.dma("sp", modv[i].rearrange("j s d -> s j d"), vec[:], reads=[r_vec], writes=[r_modv])
                P.barrier()

            def load_vec(tile, rtile, src_row):
                P.dma("sp", tile[:], src_row.partition_broadcast(128), reads=[self.r_modv], writes=[rtile])

            def rstd_for(tiles, Hjunk):
                for ti in tiles:
                    self.act(Hjunk(ti), X[:, ti, :], AF.Square, [rX[ti]], [r_stat], accum_out=stat[:, 0, ti:ti + 1])
                self.act(stat[:, 1, :], stat[:, 0, :], AF.Sqrt, [r_stat, rK], [r_stat], scale=1.0 / D, bias=epsc[:, 0:1])
                P.op("dve", lambda e: e.reciprocal(out=stat[:, 2, :], in_=stat[:, 1, :]), [r_stat], [r_stat])

            all_tiles = list(range(NT))

            with ExitStack() as s1:
                H = self.sb(s1, "H", [128, NT, D], BF16)
                rH = [P.res(f"H{i}") for i in range(NT)]
                MV = [[self.sb(s1, f"mv{s}{j}", [128, D], F32) for j in range(2)] for s in range(2)]
                rMV = [[P.res(f"mv{s}{j}") for j in range(2)] for s in range(2)]
                tmpf = self.sb(s1, "tmpf", [128, D], F32)
                r_tmpf = P.res("tmpf")
                pblk = self.sb(s1, "pblk", [128, NB, 128], BF16)
                invc = self.sb(s1, "invc", [128, NT, 4], F32)
                wp = self.sb(s1, "wp", [128, 4, 2, 256], BF16)
                ut = self.sb(s1, "ut", [128, 2, 2, 128], BF16)
                ptmp = self.sb(s1, "ptmp", [128, 2, 256], F32)
                r_pk = P.res("poolconst")
                r_ut = [P.res("ut0"), P.res("ut1")]
                r_pt = [P.res("pt0"), P.res("pt1")]
                P.dma("sp", pblk[:], k_pblk, writes=[r_pk])
                P.dma("sp", invc[:], k_invcnt, writes=[r_pk])
                P.dma("pool", wp[:], pool_w[0].rearrange("g (k p) d -> p g k d", p=128), writes=[r_pk])
                for s in range(2):
                    load_vec(MV[s][0], rMV[s][0], modv[0, 0, s:s + 1, :])
                    load_vec(MV[s][1], rMV[s][1], modv[0, 1, s:s + 1, :])
                for ti in all_tiles:
                    self.act(H[:, ti, :], X[:, ti, :], AF.Square, [rX[ti]], [r_stat, rH[ti]], accum_out=stat[:, 0, ti:ti + 1])
                self.act(stat[:, 1, :], stat[:, 0, :], AF.Sqrt, [r_stat, rK], [r_stat], scale=1.0 / D, bias=epsc[:, 0:1])
                P.op("dve", lambda e: e.reciprocal(out=stat[:, 2, :], in_=stat[:, 1, :]), [r_stat], [r_stat])
                for ti in all_tiles:
                    s = 0 if ti < NL else 1
                    self.stt("dve", tmpf[:], X[:, ti, :], stat[:, 2, ti:ti + 1], MV[s][0][:], ALU.mult, ALU.mult,
                             [rX[ti], r_stat, rMV[s][0]], [r_tmpf])
                    self.tt("dve", H[:, ti, :], tmpf[:], MV[s][1][:], ALU.add, [r_tmpf, rMV[s][1]], [rH[ti]])
                self.tap("h0", H[:], [128, NT, D], rH, BF16)
                for s in range(2):
                    load_vec(MV[s][0], rMV[s][0], modv[0, 2, s:s + 1, :])
                cnt = 0
                for j in range(4):
                    for io in range(NT):
                        s = 0 if io < NL else 1
                        if s == 0:
                            nb_ = [(ii, lat_map[(j, io, ii)]) for ii in range(NL) if (j, io, ii) in lat_map]
                        else:
                            nb_ = [(NL + ii, ctx_map[(j, io - NL, ii)]) for ii in range(2) if (j, io - NL, ii) in ctx_map]
                        b1 = cnt % 2
                        b2 = 2 + cnt % 2
                        u = cnt % 2
                        cnt += 1
                        for c in range(2):
                            ch = 2 * j + c
                            for n, (ii, bid) in enumerate(nb_):
                                self.mm(PS[b1][:, c * 128:(c + 1) * 128], H[:, ii, ch * 128:(ch + 1) * 128], pblk[:, bid, :],
                                        n == 0, n == len(nb_) - 1, [rH[ii], r_pk], [rPS[b1]])
                        self.cp("act", ut[:, u, :, :].rearrange("p c t -> p (c t)"), PS[b1][:, 0:256], [rPS[b1]], [r_ut[u]])
                        for c in range(2):
                            self.mm(PS[b2][:, 0:256], ut[:, u, c, :], wp[:, j, c, :], c == 0, c == 1, [r_ut[u], r_pk], [rPS[b2]])
                        self.stt("dve", ptmp[:, u, :], PS[b2][:, 0:256], invc[:, io, j:j + 1], MV[s][0][:, j * 256:(j + 1) * 256],
                                 ALU.mult, ALU.mult, [rPS[b2], r_pk, rMV[s][0]], [r_pt[u]])
                        self.tt("dve", X[:, io, j * 256:(j + 1) * 256], X[:, io, j * 256:(j + 1) * 256], ptmp[:, u, :], ALU.add,
                                [r_pt[u], rX[io]], [rX[io]])
                P.barrier()
            self.tap("xmix0", X[:], [128, NT, D], rX)

            def moe(layer, with_ctx):
                tiles = all_tiles if with_ctx else list(range(NL))
                nt_ = len(tiles)
                n_exp = cfg.get("n_exp", E)
                tot = SEQ + (CTX if with_ctx else 0)
                with ExitStack() as s2:
                    H = self.sb(s2, "Hm", [128, NT, D], BF16)
                    rH = [P.res(f"Hm{i}") for i in range(NT)]
                    slotb = self.sb(s2, "slotb", [16, SEQ + CTX], BF16)
                    affTb = self.sb(s2, "affTb", [16, SEQ + CTX], BF16)
                    slotT = self.sb(s2, "slotT", [128, NT, E], F32)
                    onehot = self.sb(s2, "onehot", [16, 16, 128], BF16)
                    P.dma("sp", onehot[:], k_onehot, writes=[rK])
                    r_slotb, r_affTb, r_slotT = [P.res(n) for n in "slotb affTb slotT".split()]
                    with ExitStack() as s3:
                        MV = [[self.sb(s3, f"mw{s}{j}", [128, D], F32) for j in range(2)] for s in range(2)]
                        rMV = [[P.res(f"mw{s}{j}") for j in range(2)] for s in range(2)]
                        tmpf2 = [self.sb(s3, f"tmpg{i}", [128, D], F32) for i in range(2)]
                        r_tmpf2 = [P.res("tmpg0"), P.res("tmpg1")]
                        wr = self.sb(s3, "wr", [128, 8, E], F32)
                        hT2 = [self.sb(s3, "hTs", [128, 8, 128], F32)] * 2
                        r_hT2 = [P.res("hTs")] * 2
                        logit = self.sb(s3, "logit", [128, NT, E], F32)
                        aff = self.sb(s3, "aff", [128, NT, E], F32)
                        sm = self.sb(s3, "sm", [128, 4, NT], F32)
                        affT = self.sb(s3, "affT", [16, SEQ + CTX], F32)
                        slotf = self.sb(s3, "slotf", [16, SEQ + CTX], F32)
                        mx8 = self.sb(s3, "mx8", [16, 8], F32)
                        r_wr, r_logit, r_aff, r_sm, r_affT, r_mx8, r_slotf = [
                            P.res(n) for n in "wr logit aff sm affT mx8 slotf".split()]
                        P.dma("sp", wr[:], router[layer].rearrange("(k p) e -> p k e", p=128), writes=[r_wr])
                        for s in range(2 if with_ctx else 1):
                            load_vec(MV[s][0], rMV[s][0], modv[layer, 3, s:s + 1, :])
                            load_vec(MV[s][1], rMV[s][1], modv[layer, 4, s:s + 1, :])
                        for ti in tiles:
                            self.act(H[:, ti, :], X[:, ti, :], AF.Square, [rX[ti]], [r_stat, rH[ti]], accum_out=stat[:, 0, ti:ti + 1])
                        self.act(stat[:, 1, :], stat[:, 0, :], AF.Sqrt, [r_stat, rK], [r_stat], scale=1.0 / D, bias=epsc[:, 0:1])
                        P.op("dve", lambda e: e.reciprocal(out=stat[:, 2, :], in_=stat[:, 1, :]), [r_stat], [r_stat])
                        for ti in tiles:
                            s = 0 if ti < NL else 1
                            u = ti % 2
                            tmpf = tmpf2[u]
                            r_tmpf = r_tmpf2[u]
                            hT = hT2[u]
                            r_hT = r_hT2[u]
                            pb = 4 * u
                            self.stt("dve", tmpf[:], X[:, ti, :], stat[:, 2, ti:ti + 1], MV[s][0][:], ALU.mult, ALU.mult,
                                     [rX[ti], r_stat, rMV[s][0]], [r_tmpf])
                            self.tt("dve", tmpf[:], tmpf[:], MV[s][1][:], ALU.add, [r_tmpf, rMV[s][1]], [r_tmpf])
                            self.cp("act", H[:, ti, :], tmpf[:], [r_tmpf], [rH[ti]])
                            for k in range(8):
                                b = pb + k // 4
                                self.tr(PS[b][:, (k % 4) * 128:(k % 4 + 1) * 128], tmpf[:, k * 128:(k + 1) * 128], identf[:],
                                        [r_tmpf, rK], [rPS[b]])
                            for b in range(2):
                                self.cp("act", hT[:, b * 4:(b + 1) * 4, :].rearrange("p k t -> p (k t)"), PS[pb + b][:, :], [rPS[pb + b]], [r_hT])
                            for k in range(8):
                                self.mm(PS[pb + 2][:, 0:E], hT[:, k, :], wr[:, k, :], k == 0, k == 7, [r_hT, r_wr], [rPS[pb + 2]])
                            self.cp("dve", logit[:, ti, :], PS[pb + 2][:, 0:E], [rPS[pb + 2]], [r_logit])
                        P.op("dve", lambda e: e.tensor_reduce(out=sm[:, 0, 0:nt_], in_=logit[:, 0:nt_, :], axis=AX.X, op=ALU.max),
                             [r_logit], [r_sm])
                        self.tt("dve", aff[:, 0:nt_, :], logit[:, 0:nt_, :], sm[:, 0, 0:nt_].unsqueeze(2).to_broadcast([128, nt_, E]),
                                ALU.subtract, [r_logit, r_sm], [r_aff])
                        self.act(aff[:, 0:nt_, :], aff[:, 0:nt_, :], AF.Exp, [r_aff], [r_aff])
                        P.op("dve", lambda e: e.tensor_reduce(out=sm[:, 1, 0:nt_], in_=aff[:, 0:nt_, :], axis=AX.X, op=ALU.add),
                             [r_aff], [r_sm])
                        P.op("dve", lambda e: e.reciprocal(out=sm[:, 2, 0:nt_], in_=sm[:, 1, 0:nt_]), [r_sm], [r_sm])
                        self.tt("dve", aff[:, 0:nt_, :], aff[:, 0:nt_, :], sm[:, 2, 0:nt_].unsqueeze(2).to_broadcast([128, nt_, E]),
                                ALU.mult, [r_aff, r_sm], [r_aff])
                        self.tap(f"aff{layer}", aff[:], [128, NT, E], [r_aff])
                        for g in range((nt_ + 3) // 4):
                            tl = tiles[g * 4:(g + 1) * 4]
                            for n, ti in enumerate(tl):
                                self.tr(PS[3][0:16, n * 128:(n + 1) * 128], aff[:, ti, :], identf[:], [r_aff, rK], [rPS[3]])
                            w_ = len(tl) * 128
                            self.cp("act", affT[:, g * 512:g * 512 + w_], PS[3][0:16, 0:w_], [rPS[3]], [r_affT])
                        self.cp("act", affTb[:, 0:tot], affT[:, 0:tot], [r_affT], [r_affTb])
                        groups = [(0, SEQ, 256)] + ([(SEQ, CTX, 32)] if with_ctx else [])
                        for (o0, n0, k0) in groups:
                            for it in range(k0 // 8):
                                P.op("dve", lambda e, o0=o0, n0=n0: e.max(out=mx8[:], in_=affT[:, o0:o0 + n0]), [r_affT], [r_mx8])
                                P.op("dve", lambda e, o0=o0, n0=n0: e.match_replace(out=affT[:, o0:o0 + n0], in_to_replace=mx8[:],
                                                                                   in_values=affT[:, o0:o0 + n0], imm_value=0.0),
                                     [r_affT, r_mx8], [r_affT])
                        self.ts("dve", affT[:, 0:tot], affT[:, 0:tot], 0.0, None, ALU.is_equal, None, [r_affT], [r_affT])
                        for (o0, n0, k0) in groups:
                            P.op("dve", lambda e, o0=o0, n0=n0: e.tensor_tensor_scan(out=slotf[:, o0:o0 + n0], data0=affT[:, o0:o0 + n0],
                                                                                     data1=affT[:, o0:o0 + n0], initial=0.0,
                                                                                     op0=ALU.add, op1=ALU.max),
                                 [r_affT], [r_slotf])
                        self.tt("dve", slotf[:, 0:tot], slotf[:, 0:tot], affT[:, 0:tot], ALU.mult, [r_slotf, r_affT], [r_slotf])
                        self.ts("dve", slotf[:, 0:tot], slotf[:, 0:tot], -1.0, None, ALU.add, None, [r_slotf], [r_slotf])
                        self.cp("dve", slotb[:, 0:tot], slotf[:, 0:tot], [r_slotf], [r_slotb])
                        for g in range((nt_ + 3) // 4):
                            tl = tiles[g * 4:(g + 1) * 4]
                            for n, ti in enumerate(tl):
                                self.tr(PS[3][:, n * 16:(n + 1) * 16], slotf[:, ti * 128:(ti + 1) * 128], identf[0:16, 0:16],
                                        [r_slotf, rK], [rPS[3]])
                            self.cp("act", slotT[:, g * 4:g * 4 + len(tl), :].rearrange("p t e -> p (t e)"), PS[3][:, 0:len(tl) * 16],
                                    [rPS[3]], [r_slotT])
                        self.tap(f"slotT{layer}", slotT[:], [128, NT, E], [r_slotT])
                        P.barrier()

                    with ExitStack() as s4:
                        GV = [self.sb(s4, f"gv{s}", [128, D], F32) for s in range(2)]
                        rGV = [P.res(f"gv{s}") for s in range(2)]
                        selL = self.sb(s4, "selL", [128, NL, 256], BF16)
                        selC = self.sb(s4, "selC", [128, 2, 32], BF16)
                        sgt = self.sb(s4, "sgt", [128, 2, SEQ], BF16)
                        sgtC = self.sb(s4, "sgtC", [128, CTX], BF16)
                        abro = [self.sb(s4, f"abro{i}", [128, 512], F32) for i in range(2)]
                        xst = [self.sb(s4, f"xst{i}", [128, 8, 288 if with_ctx else 256], BF16) for i in range(2)]
                        hid = self.sb(s4, "hid", [128, 2, 4, 288], BF16)
                        sg = self.sb(s4, "sg", [128, 2, 288], F32)
                        yb = self.sb(s4, "yb", [128, 2, D], BF16)
                        ybC = self.sb(s4, "ybC", [128, D], BF16)
                        r_selL, r_selC, r_sgtC = [P.res(n) for n in "selL selC sgtC".split()]
                        r_sgt = [[P.res(f"sgt{h_}{t_}") for t_ in range(4)] for h_ in range(2)]
                        r_yb = [[P.res(f"yb{h_}{d_}") for d_ in range(2)] for h_ in range(2)]
                        r_ybC = [P.res("ybC0"), P.res("ybC1")]
                        r_abro = [P.res("abro0"), P.res("abro1")]
                        r_xst = [[P.res(f"xst{i_}{k_}") for k_ in range(8)] for i_ in range(2)]
                        r_sg = [P.res("sg0"), P.res("sg1")]
                        r_hid = [[P.res(f"hid{h_}{c_}") for c_ in range(4)] for h_ in range(2)]
                        for s in range(2 if with_ctx else 1):
                            load_vec(GV[s], rGV[s], modv[layer, 5, s:s + 1, :])
                        S = 288 if with_ctx else 256
                        wgv = w_gate[layer]
                        wuv = w_up[layer]
                        wdv = w_down[layer]
                        halves = [(0, 128), (1, 128)] + ([(2, 32)] if with_ctx else [])
                        def sel_gather(ex):
                            xs = xst[ex % len(xst)]
                            rxs = r_xst[ex % len(xst)]
                            self.tt("dve", selL[:], iota[:].unsqueeze(1).to_broadcast([128, NL, 256]),
                                    slotT[:, 0:NL, ex:ex + 1].to_broadcast([128, NL, 256]), ALU.is_equal, [r_slotT, rK], [r_selL])
                            if with_ctx:
                                self.tt("dve", selC[:], iota[:, 0:32].unsqueeze(1).to_broadcast([128, 2, 32]),
                                        slotT[:, NL:NT, ex:ex + 1].to_broadcast([128, 2, 32]), ALU.is_equal, [r_slotT, rK], [r_selC])
                            for k in range(8):
                                b = k % 2
                                for ti in range(NL):
                                    self.mm(PS[b][:, 0:256], H[:, ti, k * 128:(k + 1) * 128], selL[:, ti, :], ti == 0, ti == NL - 1,
                                            [rH[ti], r_selL], [rPS[b]])
                                if with_ctx:
                                    for tc in range(2):
                                        self.mm(PS[b][:, 256:288], H[:, NL + tc, k * 128:(k + 1) * 128], selC[:, tc, :], tc == 0, tc == 1,
                                                [rH[NL + tc], r_selC], [rPS[b]])
                                self.cp("act", xs[:, k, 0:S], PS[b][:, 0:S], [rPS[b]], [rxs[k]])

                        def build_sgt(ex):
                            for tb in range(4):
                                b0 = 2 * (tb % 2)
                                ab = abro[tb % 2]
                                rab = r_abro[tb % 2]
                                self.mm(PS[b0][:, :], onehot[:, ex, :], slotb[:, tb * 512:(tb + 1) * 512], True, True, [r_slotb, rK], [rPS[b0]])
                                self.mm(PS[b0 + 1][:, :], onehot[:, ex, :], affTb[:, tb * 512:(tb + 1) * 512], True, True, [r_affTb, rK], [rPS[b0 + 1]])
                                self.cp("act", ab[:], PS[b0 + 1][:, :], [rPS[b0 + 1]], [rab])
                                for half in range(2):
                                    self.stt("dve", sgt[:, half, tb * 512:(tb + 1) * 512], PS[b0][:, :], iotap[:, half:half + 1], ab[:],
                                             ALU.is_equal, ALU.mult, [rPS[b0], rab, rK], [r_sgt[half][tb]])
                            if with_ctx:
                                ab = abro[0]
                                rab = r_abro[0]
                                self.mm(PS[0][:, 0:256], onehot[:, ex, :], slotb[:, SEQ:SEQ + CTX], True, True, [r_slotb, rK], [rPS[0]])
                                self.mm(PS[1][:, 0:256], onehot[:, ex, :], affTb[:, SEQ:SEQ + CTX], True, True, [r_affTb, rK], [rPS[1]])
                                self.cp("act", ab[:, 0:256], PS[1][:, 0:256], [rPS[1]], [rab])
                                self.stt("dve", sgtC[:, :], PS[0][:, 0:256], iotap[:, 0:1], ab[:, 0:256],
                                         ALU.is_equal, ALU.mult, [rPS[0], rab, rK], [r_sgtC])

                        pipelined = cfg.get("pipeline", True)
                        for ex in range(n_exp):
                            if ex == 0 or not pipelined:
                                sel_gather(ex)
                            build_sgt(ex)
                            xs = xst[ex % len(xst)]
                            rxs = r_xst[ex % len(xst)]
                            for fb in range(4):
                                hb = fb % 2
                                sg_, rg_ = ring_next()
                                g3 = sg_[:].rearrange("p (k n) -> p k n", k=8)
                                P.dma("pool", g3, wgv[ex].rearrange("(k p) n -> p k n", p=128)[:, :, fb * 512:(fb + 1) * 512], writes=[rg_])
                                su_, ru_ = ring_next()
                                u3 = su_[:].rearrange("p (k n) -> p k n", k=8)
                                P.dma("pool", u3, wuv[ex].rearrange("(k p) n -> p k n", p=128)[:, :, fb * 512:(fb + 1) * 512], writes=[ru_])
                                sd_, rd_ = ring_next()
                                d3 = sd_[:].rearrange("p (k n) -> p k n", k=4)
                                P.dma("pool", d3, wdv[ex].rearrange("(k p) n -> p k n", p=128)[:, fb * 4:(fb + 1) * 4, :], writes=[rd_])
                                for fc in range(4):
                                    for k in range(8):
                                        self.mm(PS[2][:, 0:S], g3[:, k, fc * 128:(fc + 1) * 128], xs[:, k, 0:S], k == 0, k == 7,
                                                [rg_, rxs[k]], [rPS[2]])
                                    self.act(sg[:, fc % 2, 0:S], PS[2][:, 0:S], AF.Silu, [rPS[2]], [r_sg[fc % 2]])
                                    for k in range(8):
                                        self.mm(PS[3][:, 0:S], u3[:, k, fc * 128:(fc + 1) * 128], xs[:, k, 0:S], k == 0, k == 7,
                                                [ru_, rxs[k]], [rPS[3]])
                                    self.tt("dve", hid[:, hb, fc, 0:S], sg[:, fc % 2, 0:S], PS[3][:, 0:S], ALU.mult,
                                            [r_sg[fc % 2], rPS[3]], [r_hid[hb][fc]])
                                if pipelined and fb == 0 and ex + 1 < n_exp:
                                    sel_gather(ex + 1)
                                for fc in range(4):
                                    f = fb * 4 + fc
                                    for (hh, hn) in halves:
                                        for dh in range(2):
                                            if hh < 2:
                                                o_ = PS[4 + hh * 2 + dh][:, :]
                                                ro_ = rPS[4 + hh * 2 + dh]
                                            else:
                                                o_ = PS[dh][0:32, :]
                                                ro_ = rPS[dh]
                                            self.mm(o_, hid[:, hb, fc, hh * 128:hh * 128 + hn], d3[:, fc, dh * 512:(dh + 1) * 512],
                                                    f == 0, f == 15, [r_hid[hb][fc], rd_], [ro_])
                            for (hh, hn) in halves:
                                for dh in range(2):
                                    if hh < 2:
                                        self.tt("dve", yb[:, hh, dh * 512:(dh + 1) * 512], PS[4 + hh * 2 + dh][:, :], GV[0][:, dh * 512:(dh + 1) * 512],
                                                ALU.mult, [rPS[4 + hh * 2 + dh], rGV[0]], [r_yb[hh][dh]])
                                    else:
                                        self.tt("dve", ybC[0:32, dh * 512:(dh + 1) * 512], PS[dh][0:32, :], GV[1][0:32, dh * 512:(dh + 1) * 512],
                                                ALU.mult, [rPS[dh], rGV[1]], [r_ybC[dh]])
                            n_sc = 0
                            for dh in range(2):
                                for ti in tiles:
                                    b = n_sc % 4
                                    n_sc += 1
                                    if ti < NL:
                                        for hh in range(2):
                                            self.mm(PS[b][:, :], sgt[:, hh, ti * 128:(ti + 1) * 128], yb[:, hh, dh * 512:(dh + 1) * 512],
                                                    hh == 0, hh == 1, [r_sgt[hh][ti // 4], r_yb[hh][dh]], [rPS[b]])
                                    else:
                                        tc = ti - NL
                                        self.mm(PS[b][:, :], sgtC[0:32, tc * 128:(tc + 1) * 128], ybC[0:32, dh * 512:(dh + 1) * 512],
                                                True, True, [r_sgtC, r_ybC[dh]], [rPS[b]])
                                    self.tt("dve", X[:, ti, dh * 512:(dh + 1) * 512], X[:, ti, dh * 512:(dh + 1) * 512], PS[b][:, :], ALU.add,
                                            [rPS[b], rX[ti]], [rX[ti]])
                        P.barrier()

            if cfg.get("do_moe0", True):
                moe(0, True)
            self.tap("xmoe0", X[:], [128, NT, D], rX)

            def mlstm():
                h_sc = 0.125
                with ExitStack() as m0:
                    hTm = self.sb(m0, "hTm", [128, 8, NT * 128], BF16)
                    r_hTm = [P.res(f"hTm{i}") for i in range(NT)]
                    tri = self.sb(m0, "tri", [128, 3, 128], F32)
                    mask2 = self.sb(m0, "mask2", [128, 2, 256], F32)
                    onec = self.sb(m0, "onec", [128, 1], F32)
                    EQ = self.sb(m0, "EQ", [128, NT, 2, 8], F32)
                    EK = self.sb(m0, "EK", [128, NT, 2, 8], F32)
                    EGC = self.sb(m0, "EGC", [128, NT, 2, 4], F32)
                    G1 = self.sb(m0, "G1t", [128, D], F32)
                    NG = self.sb(m0, "NGt", [128, D], F32)
                    bgt = self.sb(m0, "bgt", [128, 32], F32)
                    wgt = self.sb(m0, "wgt", [128, 8, 32], BF16)
                    rT = P.res("mconst", const=True)
                    r_gates, r_SP, r_CS, r_EQ, r_EK, r_EGt, r_EGC = [P.res(n) for n in "gates SP CS EQ EK EGt EGC".split()]
                    P.dma("sp", tri[:], k_tri, writes=[rT])
                    P.dma("sp", mask2[:], k_mask2, writes=[rT])
                    P.op("dve", lambda e: e.memset(onec[:], 1.0), writes=[rT])
                    P.dma("sp", bgt[:], b_gates[0:1, :].partition_broadcast(128), writes=[rT])
                    P.dma("pool", wgt[:], w_in[0].rearrange("(k p) n -> p k n", p=128)[:, :, 3072:3104], writes=[rT])
                    load_vec(G1, rT, modv[1, 2, 0:1, :])
                    P.dma("sp", NG[:], m_norm[0:1, :].partition_broadcast(128), writes=[rT])
                    with ExitStack() as m1:
                        MV = [[self.sb(m1, f"mz{s}{j}", [128, D], F32) for j in range(2)] for s in range(2)]
                        rMV = [[P.res(f"mz{s}{j}") for j in range(2)] for s in range(2)]
                        tmpf = self.sb(m1, "tmph", [128, D], F32)
                        r_tmpf = P.res("tmph")
                        hb = self.sb(m1, "hb", [128, 2, D], BF16)
                        r_hb = [P.res("hb0"), P.res("hb1")]
                        jb = self.sb(m1, "jb", [128, D], BF16)
                        r_jb = P.res("jb")
                        for s in range(2):
                            load_vec(MV[s][0], rMV[s][0], modv[1, 0, s:s + 1, :])
                            load_vec(MV[s][1], rMV[s][1], modv[1, 1, s:s + 1, :])
                        for ti in all_tiles:
                            self.act(jb[:], X[:, ti, :], AF.Square, [rX[ti]], [r_stat, r_jb], accum_out=stat[:, 0, ti:ti + 1])
                        self.act(stat[:, 1, :], stat[:, 0, :], AF.Sqrt, [r_stat, rK], [r_stat], scale=1.0 / D, bias=epsc[:, 0:1])
                        P.op("dve", lambda e: e.reciprocal(out=stat[:, 2, :], in_=stat[:, 1, :]), [r_stat], [r_stat])
                        for ti in all_tiles:
                            s = 0 if ti < NL else 1
                            u = ti % 2
                            self.stt("dve", tmpf[:], X[:, ti, :], stat[:, 2, ti:ti + 1], MV[s][0][:], ALU.mult, ALU.mult,
                                     [rX[ti], r_stat, rMV[s][0]], [r_tmpf])
                            self.tt("dve", hb[:, u, :], tmpf[:], MV[s][1][:], ALU.add, [r_tmpf, rMV[s][1]], [r_hb[u]])
                            psb = PS[u][:, :].bitcast(BF16)
                            for k in range(8):
                                self.tr(psb[:, k * 128:(k + 1) * 128], hb[:, u, k * 128:(k + 1) * 128], identb[:], [r_hb[u], rK], [rPS[u]])
                            self.cp("act", hTm[:, :, ti * 128:(ti + 1) * 128], psb.rearrange("p (k t) -> p k t", k=8), [rPS[u]], [r_hTm[ti]])
                        P.barrier()
                    if cfg.get("ml_stop", 9) <= 1:
                        P.barrier()
                        return
                    m2 = ExitStack()
                    gates = self.sb(m2, "gates", [128, NT, 32], F32)
                    SP = self.sb(m2, "SP", [128, NT, 2, 8], F32)
                    CS = self.sb(m2, "CS", [128, NT, 32], F32)
                    EGt = self.sb(m2, "EGt", [128, NT, 16], F32)
                    for ti in all_tiles:
                        b = 2 + ti // 9
                        off = (ti % 9) * 32
                        for k in range(8):
                            self.mm(PS[b][:, off:off + 32], hTm[:, k, ti * 128:(ti + 1) * 128], wgt[:, k, :], k == 0, k == 7,
                                    [r_hTm[ti], rT], [rPS[b]])
                    for b in range(2):
                        self.tt("dve", gates[:, 9 * b:9 * b + 9, :], PS[2 + b][:, 0:288].rearrange("p (t g) -> p t g", g=32),
                                bgt[:].unsqueeze(1).to_broadcast([128, 9, 32]), ALU.add, [rPS[2 + b], rT], [r_gates])
                    for d in range(2):
                        self.act(SP[:, :, d, :], gates[:, :, 8 + 16 * d:16 + 16 * d], AF.Exp, [r_gates], [r_SP], scale=-1.0)
                    self.act(SP[:], SP[:], AF.Ln, [r_SP, rT], [r_SP], bias=onec[:, 0:1])
                    for ti in all_tiles:
                        b = 4 + ti // 9
                        off = (ti % 9) * 32
                        self.mm(PS[b][:, off:off + 8], tri[:, 0, :], SP[:, ti, 0, :], True, True, [r_SP, rT], [rPS[b]])
                        self.mm(PS[b][:, off + 8:off + 16], tri[:, 1, :], SP[:, ti, 1, :], True, True, [r_SP, rT], [rPS[b]])
                        self.mm(PS[b][:, off + 16:off + 32], tri[:, 2, :], SP[:, ti, :, :].rearrange("p d h -> p (d h)"), True, True,
                                [r_SP, rT], [rPS[b]])
                    for b in range(2):
                        self.cp("dve", CS[:, 9 * b:9 * b + 9, :], PS[4 + b][:, 0:288].rearrange("p (t g) -> p t g", g=32), [rPS[4 + b]], [r_CS])
                    self.act(EQ[:].rearrange("p t d h -> p t (d h)"), CS[:, :, 0:16], AF.Exp, [r_CS], [r_EQ], scale=-1.0)
                    for d in range(2):
                        self.tt("dve", EK[:, :, d, :], gates[:, :, 16 * d:16 * d + 8], CS[:, :, 8 * d:8 * d + 8], ALU.add, [r_gates, r_CS], [r_EK])
                    self.act(EK[:], EK[:], AF.Exp, [r_EK], [r_EK])
                    self.ts("dve", EK[:], EK[:], h_sc, None, ALU.mult, None, [r_EK], [r_EK])
                    self.act(EGt[:], CS[:, :, 16:32], AF.Exp, [r_CS], [r_EGt], scale=-1.0)
                    eg5 = EGt[:].rearrange("p t (d g l) -> p t d g l", d=2, g=4, l=2)
                    self.cp("dve", EGC[0:64], eg5[0:64, :, :, :, 0], [r_EGt], [r_EGC])
                    self.cp("dve", EGC[64:128], eg5[64:128, :, :, :, 1], [r_EGt], [r_EGC])
                    self.tap("mgates", gates[:], [128, NT, 32], [r_gates])
                    self.tap("mEQ", EQ[:], [128, NT, 2, 8], [r_EQ])
                    self.tap("mEK", EK[:], [128, NT, 2, 8], [r_EK])
                    self.tap("mEGC", EGC[:], [128, NT, 2, 4], [r_EGC])
                    P.barrier()
                    m2.close()
                    if cfg.get("ml_stop", 9) <= 2:
                        return
                    with ExitStack() as m3:
                        Qs = self.sb(m3, "Qs", [128, NT, 128], BF16)
                        Ks = self.sb(m3, "Ks", [128, NT, 128], BF16)
                        Vh = self.sb(m3, "Vh", [128, NT, 2, 144], BF16)
                        HF = self.sb(m3, "HF", [128, NL, 256], F32)
                        r_Q = [P.res(f"Q{i}") for i in range(NT)]
                        r_V = [P.res(f"V{i}") for i in range(NT)]
                        r_HF = [P.res(f"HF{i}") for i in range(NL)]
                        qs = [self.sb(m3, f"qs{d}", [128, 128], BF16) for d in range(2)]
                        ks = [self.sb(m3, f"ks{d}", [128, 128], BF16) for d in range(2)]
                        qkT = [self.sb(m3, f"qkT{d}", [128, 3, 128], BF16) for d in range(2)]
                        ST = [self.sb(m3, f"ST{d}", [128, 2, 128], BF16) for d in range(2)]
                        Cf = [self.sb(m3, f"Cf{d}", [128, 144], F32) for d in range(2)]
                        Cb = [self.sb(m3, f"Cb{d}", [128, 144], BF16) for d in range(2)]
                        ctmp = [self.sb(m3, f"ctmp{d}", [128, 144], F32) for d in range(2)]
                        rr = [self.sb(m3, f"rr{d}", [128, 4], F32) for d in range(2)]
                        r_qs, r_ks, r_ST, r_Cf, r_Cb, r_rr = [[P.res(f"{n}{d}") for d in range(2)]
                                                              for n in "qs ks ST Cf Cb rr".split()]
                        r_qkT = [[P.res(f"qkT{d}{a}") for a in range(3)] for d in range(2)]
                        r_ctmp = [[P.res(f"ctmp{d}{a}") for a in range(2)] for d in range(2)]
                        r_rrA = [[P.res(f"rrA{d}{a}") for a in range(2)] for d in range(2)]
                        og = self.sb(m3, "og", [128, 2, 256], F32)
                        sq = [self.sb(m3, f"sq{i}", [128, 256], F32) for i in range(2)]
                        ss = [self.sb(m3, f"ss{i}", [128, 8], F32) for i in range(2)]
                        t1 = [self.sb(m3, "t1s", [128, 256], F32)] * 2
                        ho = [self.sb(m3, f"ho{i}", [128, 256], BF16) for i in range(2)]
                        hoT = [self.sb(m3, f"hoT{i}", [128, 2, 128], BF16) for i in range(2)]
                        y5 = [self.sb(m3, "y5s", [128, 1, 512], F32)] * 2
                        r_og = [P.res("og0"), P.res("og1")]
                        r_sq, r_ss, r_ho, r_hoT = [[P.res(f"{n}{i}") for i in range(2)] for n in "sq ss ho hoT".split()]
                        r_t1 = [P.res("t1s")] * 2
                        r_y5 = [P.res("y5s")] * 2
                        for ti in all_tiles:
                            P.op("dve", lambda e, ti=ti: e.memset(Vh[:, ti, :, :].rearrange("p h v -> p (h v)"), 0.0), writes=[r_V[ti]])
                            for hl in range(2):
                                P.op("dve", lambda e, ti=ti, hl=hl: e.memset(Vh[:, ti, hl, 128:129], 1.0), writes=[r_V[ti]])
                        w3 = w_in[0].rearrange("(k p) n -> p k n", p=128)
                        order = [[16, 17] + list(range(16)), [17, 16] + list(range(15, -1, -1))]
                        for hg in range(cfg.get("n_hg", 4)):
                            slot, rs = ring_next()
                            wqkv = slot[:].rearrange("p (k n) -> p k n", k=8)
                            P.dma("pool", wqkv[:, :, 0:128], w3[:, :, hg * 128:(hg + 1) * 128], writes=[rs])
                            P.dma("pool", wqkv[:, :, 128:256], w3[:, :, 512 + hg * 128:512 + (hg + 1) * 128], writes=[rs])
                            P.dma("pool", wqkv[:, :, 256:512], w3[:, :, 1024 + hg * 256:1024 + (hg + 1) * 256], writes=[rs])
                            pj = cfg.get("pj_upto", 9)
                            for ti in all_tiles:
                                b = 4 + ti % 2
                                if pj < 2:
                                    break
                                for k in range(8):
                                    self.mm(PS[b][:, :], hTm[:, k, ti * 128:(ti + 1) * 128], wqkv[:, k, :], k == 0, k == 7,
                                            [r_hTm[ti], rs], [rPS[b]])
                                if pj < 3:
                                    continue
                                self.cp("act", Qs[:, ti, :], PS[b][:, 0:128], [rPS[b]], [r_Q[ti]])
                                self.cp("act", Ks[:, ti, :], PS[b][:, 128:256], [rPS[b]], [r_Q[ti]])
                                if pj < 4:
                                    continue
                                for hl in range(2):
                                    self.cp("act", Vh[:, ti, hl, 0:128], PS[b][:, 256 + 128 * hl:384 + 128 * hl], [rPS[b]], [r_V[ti]])
                            for ti in range(NL):
                                P.op("dve", lambda e, ti=ti: e.memset(HF[:, ti, :], 0.0), writes=[r_HF[ti]])
                            for d in range(2):
                                if hg == 0:
                                    P.op("dve", lambda e, d=d: e.memset(qkT[d][:].rearrange("p a t -> p (a t)"), 0.0), writes=r_qkT[d])
                                P.op("dve", lambda e, d=d: e.memset(Cf[d][:], 0.0), writes=[r_Cf[d]])
                                P.op("dve", lambda e, d=d: e.memset(Cb[d][:], 0.0), writes=[r_Cb[d]])
                            h0 = 2 * hg
                            if cfg.get("ml_stop", 9) <= 3:
                                P.barrier()
                                return
                            for step in range(cfg.get("ml_steps", NT)):
                                for d in range(2):
                                    c = order[d][step]
                                    lat_c = c < NL
                                    self.tt("dve", ks[d][:].rearrange("p (h k) -> p h k", h=2), Ks[:, c, :].rearrange("p (h k) -> p h k", h=2),
                                            EK[:, c, d, h0:h0 + 2].unsqueeze(2).to_broadcast([128, 2, 64]), ALU.mult,
                                            [r_Q[c], r_EK], [r_ks[d]])
                                    if lat_c:
                                        self.tt("dve", qs[d][:].rearrange("p (h k) -> p h k", h=2), Qs[:, c, :].rearrange("p (h k) -> p h k", h=2),
                                                EQ[:, c, d, h0:h0 + 2].unsqueeze(2).to_broadcast([128, 2, 64]), ALU.mult,
                                                [r_Q[c], r_EQ], [r_qs[d]])
                                        psb = PS[d][:, :].bitcast(BF16)
                                        self.tr(psb[:, 0:128], qs[d][:], identb[:], [r_qs[d], rK], [rPS[d]])
                                        self.tr(psb[:, 128:256], ks[d][:], identb[:], [r_ks[d], rK], [rPS[d]])
                                        self.cp("act", qkT[d][0:64, 0, :], psb[0:64, 0:128], [rPS[d]], [r_qkT[d][0]])
                                        self.cp("act", qkT[d][64:128, 1, :], psb[64:128, 0:128], [rPS[d]], [r_qkT[d][1]])
                                        self.cp("act", qkT[d][:, 2, :], psb[:, 128:256], [rPS[d]], [r_qkT[d][2]])
                                        for hl in range(2):
                                            self.mm(PS[2 + d][:, hl * 128:(hl + 1) * 128], qkT[d][:, 2, :],
                                                    qkT[d][:, hl, :], True, True, [r_qkT[d][2], r_qkT[d][hl]], [rPS[2 + d]])
                                        self.tt("dve", ST[d][:].rearrange("p h t -> p (h t)"), PS[2 + d][:, 0:256], mask2[:, d, :], ALU.mult,
                                                [rPS[2 + d], rT], [r_ST[d]])
                                        for hl in range(2):
                                            self.mm(PS[4 + d][:, hl * 144:hl * 144 + 129], qkT[d][:, hl, :],
                                                    Cb[d][:, 0:129], True, False, [r_qkT[d][hl], r_Cb[d]], [rPS[4 + d]])
                                            self.mm(PS[4 + d][:, hl * 144:hl * 144 + 129], ST[d][:, hl, :], Vh[:, c, hl, 0:129], False, True,
                                                    [r_ST[d], r_V[c]], [rPS[4 + d]])
                                        for hl in range(2):
                                            self.act(rr[d][:, hl:hl + 1], PS[4 + d][:, hl * 144 + 128:hl * 144 + 129], AF.Abs, [rPS[4 + d]], [r_rrA[d][hl]])
                                        self.ts("dve", rr[d][:, 0:2], rr[d][:, 0:2], 1.0, None, ALU.max, None, [r_rrA[d][0], r_rrA[d][1], r_rr[d]],
                                                [r_rr[d], r_rrA[d][0], r_rrA[d][1]])
                                        P.op("dve", lambda e, d=d: e.reciprocal(out=rr[d][:, 2:4], in_=rr[d][:, 0:2]),
                                             [r_rr[d], r_rrA[d][0], r_rrA[d][1]], [r_rr[d]])
                                        for hl in range(2):
                                            self.stt("dve", HF[:, c, hl * 128:(hl + 1) * 128], PS[4 + d][:, hl * 144:hl * 144 + 128],
                                                     rr[d][:, 2 + hl:3 + hl], HF[:, c, hl * 128:(hl + 1) * 128], ALU.mult, ALU.add,
                                                     [rPS[4 + d], r_rr[d], r_HF[c]], [r_HF[c]])
                                    self.mm(PS[6 + d][:, 0:288], ks[d][:], Vh[:, c, :, :].rearrange("p h v -> p (h v)"), True, True,
                                            [r_ks[d], r_V[c]], [rPS[6 + d]])
                                    self.tt("dve", ctmp[d][0:64, :], Cf[d][0:64, :], PS[6 + d][0:64, 0:144], ALU.add, [r_Cf[d], rPS[6 + d]], [r_ctmp[d][0]])
                                    self.tt("dve", ctmp[d][64:128, :], Cf[d][64:128, :], PS[6 + d][64:128, 144:288], ALU.add,
                                            [r_Cf[d], rPS[6 + d]], [r_ctmp[d][1]])
                                    self.ts("dve", Cf[d][:], ctmp[d][:], EGC[:, c, d, hg:hg + 1], None, ALU.mult, None,
                                            [r_ctmp[d][0], r_ctmp[d][1], r_EGC], [r_Cf[d]])
                                    self.cp("act", Cb[d][:], Cf[d][:], [r_Cf[d]], [r_Cb[d]])
                            if hg == 0:
                                self.tap("mHF", HF[:], [128, NL, 256], r_HF)
                            if cfg.get("ml_stop", 9) <= 4:
                                P.barrier()
                                return
                            slot2, rs2 = ring_next()
                            wo = slot2[:, 0:2048].rearrange("p (k n) -> p k n", k=8)
                            wout = slot2[:, 2048:4096].rearrange("p (k n) -> p k n", k=2)
                            P.dma("pool", wo, w3[:, :, 2048 + hg * 256:2048 + (hg + 1) * 256], writes=[rs2])
                            P.dma("pool", wout, w_out[0].rearrange("(k p) n -> p k n", p=128)[:, 2 * hg:2 * hg + 2, :], writes=[rs2])
                            for ti in range(NL):
                                u = ti % 2
                                bo = 4 * u
                                for k in range(8):
                                    self.mm(PS[bo][:, 0:256], hTm[:, k, ti * 128:(ti + 1) * 128], wo[:, k, :], k == 0, k == 7,
                                            [r_hTm[ti], rs2], [rPS[bo]])
                                self.act(og[:, u, :], PS[bo][:, 0:256], AF.Sigmoid, [rPS[bo]], [r_og[u]])
                                self.tt("dve", sq[u][:], HF[:, ti, :], HF[:, ti, :], ALU.mult, [r_HF[ti]], [r_sq[u]])
                                P.op("dve", lambda e, u=u: e.tensor_reduce(out=ss[u][:, 0:2], in_=sq[u][:].rearrange("p (h v) -> p h v", h=2),
                                                                          axis=AX.X, op=ALU.add), [r_sq[u]], [r_ss[u]])
                                self.act(ss[u][:, 2:4], ss[u][:, 0:2], AF.Sqrt, [r_ss[u], rK], [r_ss[u]], scale=1.0 / 128, bias=epsc[:, 0:1])
                                P.op("dve", lambda e, u=u: e.reciprocal(out=ss[u][:, 4:6], in_=ss[u][:, 2:4]), [r_ss[u]], [r_ss[u]])
                                self.tt("dve", t1[u][:].rearrange("p (h v) -> p h v", h=2), HF[:, ti, :].rearrange("p (h v) -> p h v", h=2),
                                        ss[u][:, 4:6].unsqueeze(2).to_broadcast([128, 2, 128]), ALU.mult, [r_HF[ti], r_ss[u]], [r_t1[u]])
                                self.tt("dve", t1[u][:], t1[u][:], NG[:, hg * 256:(hg + 1) * 256], ALU.mult, [r_t1[u], rT], [r_t1[u]])
                                self.tt("dve", ho[u][:], t1[u][:], og[:, u, :], ALU.mult, [r_t1[u], r_og[u]], [r_ho[u]])
                                psb = PS[bo + 1][:, :].bitcast(BF16)
                                for c2 in range(2):
                                    self.tr(psb[:, c2 * 128:(c2 + 1) * 128], ho[u][:, c2 * 128:(c2 + 1) * 128], identb[:], [r_ho[u], rK], [rPS[bo + 1]])
                                self.cp("act", hoT[u][:].rearrange("p a t -> p (a t)"), psb[:, 0:256], [rPS[bo + 1]], [r_hoT[u]])
                                for dh in range(2):
                                    for c2 in range(2):
                                        self.mm(PS[bo + 2 + dh][:, :], hoT[u][:, c2, :], wout[:, c2, dh * 512:(dh + 1) * 512], c2 == 0, c2 == 1,
                                                [r_hoT[u], rs2], [rPS[bo + 2 + dh]])
                                    self.tt("dve", y5[u][:, 0, :], PS[bo + 2 + dh][:, :], G1[:, dh * 512:(dh + 1) * 512], ALU.mult,
                                            [rPS[bo + 2 + dh], rT], [r_y5[u]])
                                    self.tt("dve", X[:, ti, dh * 512:(dh + 1) * 512], X[:, ti, dh * 512:(dh + 1) * 512], y5[u][:, 0, :], ALU.add,
                                            [r_y5[u], rX[ti]], [rX[ti]])
                        P.barrier()

            if cfg.get("do_mlstm", True):
                mlstm()
            self.tap("xmix1", X[:], [128, NT, D], rX)
            if cfg.get("do_moe1", True):
                moe(1, False)

            with ExitStack() as s5:
                FV = self.sb(s5, "fv", [128, D], F32)
                r_FV = P.res("fv")
                fj = self.sb(s5, "fj", [128, D], BF16)
                r_fj = P.res("fj")
                P.dma("sp", FV[:], final_norm[0:1, :].partition_broadcast(128), writes=[r_FV])
                lat = list(range(NL))
                for ti in lat:
                    self.act(fj[:], X[:, ti, :], AF.Square, [rX[ti]], [r_stat, r_fj], accum_out=stat[:, 0, ti:ti + 1])
                self.act(stat[:, 1, :], stat[:, 0, :], AF.Sqrt, [r_stat, rK], [r_stat], scale=1.0 / D, bias=epsc[:, 0:1])
                P.op("dve", lambda e: e.reciprocal(out=stat[:, 2, :], in_=stat[:, 1, :]), [r_stat], [r_stat])
                ov = out_d.rearrange("(t p) d -> p t d", p=128)
                for ti in lat:
                    self.stt("dve", X[:, ti, :], X[:, ti, :], stat[:, 2, ti:ti + 1], FV[:], ALU.mult, ALU.mult,
                             [rX[ti], r_stat, r_FV], [rX[ti]])
                    P.dma("sp", ov[:, ti, :], X[:, ti, :], reads=[rX[ti]])
                P.emit(st)
        return nc


_CACHE = {}


def _layout_inputs(inputs, b):
    m = {}
    m["x"] = np.ascontiguousarray(inputs["x"][b])
    m["ctx"] = np.ascontiguousarray(inputs["ctx"][b])
    cc = np.stack([inputs["c"][b], inputs["c_ctx"]], axis=-1)
    m["cc"] = np.ascontiguousarray(cc.reshape(8, 128, 2).transpose(1, 0, 2))
    for k in ("ada_w", "ada_b", "norm_mix", "norm_ffn", "pool_w", "pool_scale", "mlstm_w_in", "mlstm_b_gates",
              "mlstm_norm", "mlstm_w_out", "moe_router", "moe_w_gate", "moe_w_up", "moe_w_down"):
        m[k] = inputs[k]
    m["final_norm"] = inputs["final_norm"].reshape(1, D)
    m.update(host_constants())
    return m


def kernel(**inputs):
    inputs = {k: np.asarray(v) for k, v in inputs.items()}
    n = 8
    bld = Builder()
    nc = bld.build()
    in_maps = [_layout_inputs(inputs, b) for b in range(n)]
    res = run_bass_kernel_spmd(nc, in_maps, core_ids=list(range(n)))
    return np.stack([r["out"] for r in res.results], axis=0).astype(np.float32)
```
